# Optimizing a Trainium2 kernel written in Bass

```python
import jax, jax.numpy as jnp
from jax import lax
import numpy as np

D_MODEL = 2048
BATCH = 8
SEQ = 2048
DEPTH = 2

N_EVEN = (DEPTH + 1) // 2
N_ODD = DEPTH // 2
EPS = 1e-6
D_PLE = 256
D_HGRN = D_MODEL // 2
HGRN_HEADS = 8
HGRN_HEAD_DIM = D_HGRN // HGRN_HEADS
CHUNK = 64
D_CONV = D_MODEL // 2
CONV_WIDTH = 3
D_IN_PROJ = 4 * D_HGRN + 3 * D_CONV
POOL_WINDOWS = (2, 4, 8, 16)
N_POOL_GROUPS = len(POOL_WINDOWS)
POOL_GROUP_DIM = D_MODEL // N_POOL_GROUPS
D_FF = 5632
N_EXPERTS = 8
TOP_K = 2
D_FF_EXPERT = 1024

kernel_name = 'hybrid_hgrn2_shortconv_pool_moe_trunk'


def rms_norm(x, gain):
    x32 = x.astype(jnp.float32)
    y = x32 * lax.rsqrt(jnp.mean(x32 * x32, axis=-1, keepdims=True) + EPS)
    return (y * gain.astype(jnp.float32)).astype(x.dtype)


def hgrn2_chunked(q, k, v, log_f):
    b_, s_, h_, dk = q.shape
    dv = v.shape[-1]
    n_chunks = s_ // CHUNK

    def to_chunks(t):
        return t.reshape(b_, n_chunks, CHUNK, h_, t.shape[-1]).transpose(0, 3, 1, 2, 4)

    q, k, v, log_f = to_chunks(q), to_chunks(k), to_chunks(v), to_chunks(log_f)
    cum = jnp.cumsum(log_f, axis=3)
    ref = cum[:, :, :, CHUNK // 2:CHUNK // 2 + 1, :]
    last = cum[:, :, :, -1:, :]
    scores = jnp.einsum('bhncd,bhnsd->bhncs', q * jnp.exp(cum - ref), k * jnp.exp(ref - cum))
    causal = jnp.tril(jnp.ones((CHUNK, CHUNK), dtype=bool))
    scores = jnp.where(causal, scores, 0.0)
    o_intra = jnp.einsum('bhncs,bhnsv->bhncv', scores, v)
    kv = jnp.einsum('bhncd,bhncv->bhndv', k * jnp.exp(last - cum), v)
    decay = jnp.exp(last[:, :, :, 0, :])

    def step(state, inp):
        dec, kv_c = inp
        return dec[..., None] * state + kv_c, state

    s0 = jnp.zeros((b_, h_, dk, dv), jnp.float32)
    _, s_prev = lax.scan(step, s0, (jnp.moveaxis(decay, 2, 0), jnp.moveaxis(kv, 2, 0)))
    o_inter = jnp.einsum('bhncd,nbhdv->bhncv', q * jnp.exp(cum), s_prev)
    o = o_intra + o_inter
    return o.transpose(0, 2, 3, 1, 4).reshape(b_, s_, h_, dv)


def hgrn2_shortconv_mixer(h, w_in, lb, hgrn_gain, conv_w, w_out):
    b_, s_, _ = h.shape
    u = h @ w_in
    q, f_raw, i_val, g, gate_b, gate_c, v_conv = jnp.split(
        u, [D_HGRN, 2 * D_HGRN, 3 * D_HGRN, 4 * D_HGRN,
            4 * D_HGRN + D_CONV, 4 * D_HGRN + 2 * D_CONV], axis=-1)

    def heads(t):
        return t.reshape(b_, s_, HGRN_HEADS, HGRN_HEAD_DIM)

    f = lb + (1.0 - lb) * jax.nn.sigmoid(f_raw.astype(jnp.float32))
    o = hgrn2_chunked(heads(jax.nn.silu(q.astype(jnp.float32))), heads(1.0 - f),
                      heads(i_val.astype(jnp.float32)), heads(jnp.log(f)))
    o = o * lax.rsqrt(jnp.mean(o * o, axis=-1, keepdims=True) + EPS)
    o = o.reshape(b_, s_, D_HGRN) * hgrn_gain.astype(jnp.float32) * jax.nn.silu(g.astype(jnp.float32))
    a_out = o.astype(h.dtype)

    t = gate_c * v_conv
    padded = jnp.pad(t, ((0, 0), (CONV_WIDTH - 1, 0), (0, 0)))
    conv = sum(padded[:, j:j + s_] * conv_w[j] for j in range(CONV_WIDTH))
    b_out = gate_b * conv

    return jnp.concatenate([a_out, b_out], axis=-1) @ w_out


def pool_mixer(h, pool_w, pool_scale):
    b_, s_, _ = h.shape
    hg = h.astype(jnp.float32).reshape(b_, s_, N_POOL_GROUPS, POOL_GROUP_DIM)
    csum = jnp.cumsum(hg, axis=1)
    pos = jnp.arange(1, s_ + 1, dtype=jnp.float32)
    diffs = []
    for gi, w in enumerate(POOL_WINDOWS):
        c = csum[:, :, gi]
        prev = jnp.pad(c, ((0, 0), (w, 0), (0, 0)))[:, :s_]
        mean = (c - prev) / jnp.minimum(pos, float(w))[None, :, None]
        diffs.append(mean - hg[:, :, gi])
    d = jnp.stack(diffs, axis=2).astype(h.dtype)
    y = jnp.einsum('bsgc,gce->bsge', d, pool_w).reshape(b_, s_, D_MODEL)
    return y * pool_scale


def swiglu(h, w_gate, w_up, w_down):
    return (jax.nn.silu(h @ w_gate) * (h @ w_up)) @ w_down


def moe_swiglu(h, w_router, w_gate_e, w_up_e, w_down_e):
    logits = (h @ w_router).astype(jnp.float32)
    probs = jax.nn.softmax(logits, axis=-1)
    top_p, top_i = lax.top_k(probs, TOP_K)
    top_p = top_p / jnp.sum(top_p, axis=-1, keepdims=True)
    gates = jnp.sum(jax.nn.one_hot(top_i, N_EXPERTS, dtype=jnp.float32) * top_p[..., None], axis=-2)
    gates = gates.astype(h.dtype)
    y = jnp.zeros_like(h)
    for e in range(N_EXPERTS):
        y = y + gates[..., e:e + 1] * swiglu(h, w_gate_e[e], w_up_e[e], w_down_e[e])
    return y


def per_layer_embedding(x, p_i, norm_gain, w_gate, w_proj):
    gate = jax.nn.sigmoid(rms_norm(x, norm_gain) @ w_gate)
    return gate * (p_i @ w_proj)


def setup_inputs(seed: int = 0) -> dict:
    key = jax.random.key(seed)
    ks = jax.random.split(key, 32)
    f32 = jnp.float32

    def nrm(k, shape, fan_in):
        return jax.random.normal(k, shape, f32) * (fan_in ** -0.5)

    def gain(k, shape):
        return 1.0 + 0.02 * jax.random.normal(k, shape, f32)

    return {
        'x': jax.random.normal(ks[0], (BATCH, SEQ, D_MODEL), f32),
        'p': jax.random.normal(ks[1], (DEPTH, BATCH, SEQ, D_PLE), f32),
        'final_norm_gain': gain(ks[2], (D_MODEL,)),
        'lb_table': 1.0 + 0.1 * jax.random.normal(ks[3], (DEPTH + 1, D_HGRN), f32),
        'mix_norm_even': gain(ks[4], (N_EVEN, D_MODEL)),
        'w_in_even': nrm(ks[5], (N_EVEN, D_MODEL, D_IN_PROJ), D_MODEL),
        'hgrn_norm_gain': gain(ks[6], (N_EVEN, D_HGRN)),
        'conv_w': nrm(ks[7], (N_EVEN, CONV_WIDTH, D_CONV), CONV_WIDTH),
        'w_out_even': nrm(ks[8], (N_EVEN, D_MODEL, D_MODEL), D_MODEL),
        'ffn_norm_even': gain(ks[9], (N_EVEN, D_MODEL)),
        'w_gate_dense': nrm(ks[10], (N_EVEN, D_MODEL, D_FF), D_MODEL),
        'w_up_dense': nrm(ks[11], (N_EVEN, D_MODEL, D_FF), D_MODEL),
        'w_down_dense': nrm(ks[12], (N_EVEN, D_FF, D_MODEL), D_FF),
        'mix_norm_odd': gain(ks[13], (N_ODD, D_MODEL)),
        'pool_w': nrm(ks[14], (N_ODD, N_POOL_GROUPS, POOL_GROUP_DIM, POOL_GROUP_DIM), POOL_GROUP_DIM),
        'pool_scale': gain(ks[15], (N_ODD, D_MODEL)),
        'ffn_norm_odd': gain(ks[16], (N_ODD, D_MODEL)),
        'w_router': nrm(ks[17], (N_ODD, D_MODEL, N_EXPERTS), D_MODEL),
        'w_gate_exp': nrm(ks[18], (N_ODD, N_EXPERTS, D_MODEL, D_FF_EXPERT), D_MODEL),
        'w_up_exp': nrm(ks[19], (N_ODD, N_EXPERTS, D_MODEL, D_FF_EXPERT), D_MODEL),
        'w_down_exp': nrm(ks[20], (N_ODD, N_EXPERTS, D_FF_EXPERT, D_MODEL), D_FF_EXPERT),
        'ple_norm': gain(ks[21], (DEPTH, D_MODEL)),
        'ple_gate_w': nrm(ks[22], (DEPTH, D_MODEL, D_MODEL), D_MODEL),
        'ple_proj': nrm(ks[23], (DEPTH, D_PLE, D_MODEL), D_PLE),
    }


def reference(x, p, final_norm_gain, lb_table, mix_norm_even, w_in_even, hgrn_norm_gain,
              conv_w, w_out_even, ffn_norm_even, w_gate_dense, w_up_dense, w_down_dense,
              mix_norm_odd, pool_w, pool_scale, ffn_norm_odd, w_router, w_gate_exp,
              w_up_exp, w_down_exp, ple_norm, ple_gate_w, ple_proj):
    lb_all = jnp.cumsum(jax.nn.softmax(lb_table.astype(jnp.float32), axis=0), axis=0)
    for i in range(DEPTH):
        j = i // 2
        if i % 2 == 0:
            x = x + hgrn2_shortconv_mixer(rms_norm(x, mix_norm_even[j]), w_in_even[j], lb_all[i],
                                          hgrn_norm_gain[j], conv_w[j], w_out_even[j])
            x = x + swiglu(rms_norm(x, ffn_norm_even[j]), w_gate_dense[j], w_up_dense[j], w_down_dense[j])
        else:
            x = x + pool_mixer(rms_norm(x, mix_norm_odd[j]), pool_w[j], pool_scale[j])
            x = x + moe_swiglu(rms_norm(x, ffn_norm_odd[j]), w_router[j], w_gate_exp[j],
                               w_up_exp[j], w_down_exp[j])
        x = x + per_layer_embedding(x, p[i], ple_norm[i], ple_gate_w[i], ple_proj[i])
    return rms_norm(x, final_norm_gain)
```

```python
import numpy as np
from contextlib import ExitStack
import concourse.bass as bass
import concourse.mybir as mybir
from concourse.bass_utils import run_bass_kernel_spmd

F32 = mybir.dt.float32
BF16 = mybir.dt.bfloat16
AF = mybir.ActivationFunctionType
ALU = mybir.AluOpType
AX = mybir.AxisListType

D = 2048
KC = 16
S_FULL = 2048
T = 1024
NTILE = T // 128
EPS = 1e-6
DFF = 5632
NEXP = 8
DFE = 1024

V_FNG, V_MNE, V_HG, V_CW, V_FNE, V_MNO, V_PS, V_FNO, V_PLN, V_LB, V_WR = 0, 16, 32, 40, 64, 80, 96, 112, 128, 160, 184
V_ID, V_CM, V_RF = 312, 440, 568
NV = 632

ENGS = ("pe", "act", "dve", "pool", "sp")


class Prog:
    def __init__(self):
        self.ops = {e: [] for e in ENGS}
        self.count = {}
        self.last_w = {}
        self.readers = {}
        self.waited = {e: {} for e in ENGS}
        self.semkeys = list(ENGS)

    def _deps(self, reads, writes):
        need = {}

        def add(k, v):
            if need.get(k, 0) < v:
                need[k] = v
        for r in reads:
            d = self.last_w.get(r)
            if d is not None:
                add(*d)
        for w in writes:
            d = self.last_w.get(w)
            if d is not None:
                add(*d)
            for k, v in self.readers.get(w, {}).items():
                add(k, v)
        return need

    def _commit(self, reads, writes, me):
        k, v = me
        for r in reads:
            rd = self.readers.setdefault(r, {})
            if rd.get(k, 0) < v:
                rd[k] = v
        for w in writes:
            self.last_w[w] = me
            self.readers[w] = {}

    def _filter(self, eng, need):
        waits = []
        for k, v in need.items():
            if k == "pe" and eng == "pe":
                continue
            if self.waited[eng].get(k, 0) >= v:
                continue
            self.waited[eng][k] = v
            waits.append((k, v))
        return waits

    def op(self, eng, fn, reads=(), writes=()):
        need = self._deps(reads, writes)
        waits = self._filter(eng, need)
        self.count[eng] = self.count.get(eng, 0) + 1
        me = (eng, self.count[eng])
        self._commit(reads, writes, me)
        self.ops[eng].append((waits, fn, (eng, 1)))
        return me

    def dma(self, eng, slot, fn, reads=(), writes=(), n=1):
        key = "dma:" + slot
        if key not in self.semkeys:
            self.semkeys.append(key)
        need = self._deps(reads, writes)
        waits = self._filter(eng, need)
        self.count[key] = self.count.get(key, 0) + 16 * n
        me = (key, self.count[key])
        self._commit(reads, writes, me)
        self.ops[eng].append((waits, fn, (key, 16)))
        return me

    def final_wait(self, eng, deps):
        self.ops[eng].append((list(deps), None, None))

    def emit(self, block, sems):
        def run(engobj, name):
            for waits, fn, inc in self.ops[name]:
                for k, v in waits:
                    engobj.wait_ge(sems[k], v)
                if fn is None:
                    continue
                r = fn(engobj)
                if isinstance(r, (list, tuple)):
                    for ins in r:
                        ins.then_inc(sems[inc[0]], inc[1])
                else:
                    r.then_inc(sems[inc[0]], inc[1])

        @block.tensor
        def _(e):
            run(e, "pe")

        @block.scalar
        def _(e):
            run(e, "act")

        @block.vector
        def _(e):
            run(e, "dve")

        @block.gpsimd
        def _(e):
            run(e, "pool")

        @block.sync
        def _(e):
            run(e, "sp")


def build(stop_after=6, n_halves=2, do_final=True):
    nc = bass.Bass("TRN2", target_bir_lowering=False)

    def din(name, shape):
        return nc.dram_tensor(name, list(shape), F32, kind="ExternalInput").ap()
    x_in = din("x_in", [S_FULL, D])
    p_in = din("p_in", [2, S_FULL, 256])
    vecs_in = din("vecs", [128, NV])
    w_in = din("w_in", [D, 7168])
    w_out = din("w_out", [D, D])
    w_gate = din("w_gate", [D, DFF])
    w_up = din("w_up", [D, DFF])
    w_down = din("w_down", [DFF, D])
    pool_w = din("pool_w", [4, 512, 512])
    w_ge = din("w_ge", [NEXP, D, DFE])
    w_ue = din("w_ue", [NEXP, D, DFE])
    w_de = din("w_de", [NEXP, DFE, D])
    ple_gw = din("ple_gw", [2, D, D])
    ple_pj = din("ple_pj", [2, 256, D])
    y_out = nc.dram_tensor("y_out", [S_FULL, D], F32, kind="ExternalOutput").ap()

    es = ExitStack()
    with es:
        def sb(name, shape, dt):
            return es.enter_context(nc.sbuf_tensor(name, list(shape), dt))
        xs = sb("xs", [128, KC, T], F32)
        hs = sb("hs", [128, KC, T], BF16)
        NWH = 8
        wb = sb("wb", [128, NWH * 2048], BF16)
        NSCR = 14
        scr = sb("scr", [128, NSCR * 1024], F32)
        rstd = sb("rstd", [128, T], F32)
        vecs = sb("vecs_sb", [128, NV], F32)
        Sst = sb("Sst", [128, 8, 128], F32)
        Sb = [sb(f"Sb{i}", [128, 128], BF16) for i in range(4)]
        Stmp = sb("Stmp", [128, 128], F32)
        Pm = [sb(f"Pm{i}", [128, 128], BF16) for i in range(2)]
        identb = sb("identb", [128, 128], BF16)
        onesb = sb("onesb", [128, 128], BF16)
        onesf = sb("onesf", [128, 128], F32)
        rmask = sb("rmask", [128, T], F32)
        lbv = sb("lbv", [128, 3, 8], F32)
        lbd = sb("lbd", [128, 4, 8], F32)
        ccar = sb("ccar", [128, 8, 2], F32)
        pcarb = sb("pcarb", [128, KC, 16], BF16)
        wrg = sb("wrg", [128, KC, 8], F32)
        lgt = sb("lgt", [128, NTILE, 8], F32)
        gsm = sb("gsm", [128, 6, NTILE, 8], F32)
        gm = sb("gm", [128, 4, NTILE], F32)
        rtk = sb("rtk", [128, NTILE], F32)
        selc = sb("selc", [8, NEXP, 128], BF16)
        psum = es.enter_context(nc.psum_tensor("psum", [128, 8, 512], F32))
        psflat = psum[:, :, :].rearrange("p b n -> p (b n)")

        P = Prog()
        ident = vecs[:, V_ID:V_ID + 128]
        cmask = vecs[:, V_CM:V_CM + 128]

        def S32(slot, n=1024, off=0):
            return scr[:, slot * 1024 + off: slot * 1024 + off + n]

        def S16(slot, half=None):
            v = scr[:, slot * 1024:(slot + 1) * 1024].bitcast(BF16)
            if half is None:
                return v
            return v[:, half * 1024:(half + 1) * 1024]

        def R(slot, half=None):
            if half is None:
                return [f"s{slot}a", f"s{slot}b"]
            return [f"s{slot}{'ab'[half]}"]

        def PS(i):
            return psflat[:, i * 1024:(i + 1) * 1024]

        def RPS(i):
            return [f"pb{2 * i}", f"pb{2 * i + 1}"]

        HR = [f"h{c}" for c in range(KC)]
        wstate = {"n": 0}

        def wload(src3, kcn, ncols, after=()):
            size = kcn * ncols
            nhs = (size + 2047) // 2048
            assert nhs in (1, 2)
            if nhs == 2 and wstate["n"] % 2 == 1:
                wstate["n"] += 1
            idx = wstate["n"] % NWH
            wstate["n"] += nhs
            dst = wb[:, idx * 2048: idx * 2048 + size].rearrange("p (k n) -> p k n", k=kcn)
            res = [f"wh{idx + i}" for i in range(nhs)]
            P.dma("pool", f"w{idx}", lambda e: [e.dma_start(out=dst, in_=src3)], reads=list(after), writes=res)
            return dst, res

        def wcols(W2, r0, nrows, c0, ncols, after=()):
            src = W2[r0:r0 + nrows, c0:c0 + ncols].rearrange("(k p) n -> p k n", p=128)
            return wload(src, nrows // 128, ncols, after=after)

        def mm_group(ps, wl, rl, reads, wres):
            n = len(wl)

            def fn(e):
                last = None
                for i in range(n):
                    for tb in range(2):
                        last = e.matmul(ps[:, tb * 512:(tb + 1) * 512], lhsT=wl[i],
                                        rhs=rl[i][:, tb * 512:(tb + 1) * 512],
                                        start=(i == 0), stop=(i == n - 1))
                return last
            P.op("pe", fn, reads=reads, writes=wres)

        def mm_pair_kpipe(psA, wlA, resA, psB, wlB, resB, rl, rnames, wres):
            n = len(rl)
            for k in range(n):
                def fn(e, k=k):
                    last = None
                    for (ps, wl) in ((psA, wlA), (psB, wlB)):
                        for tb in range(2):
                            last = e.matmul(ps[:, tb * 512:(tb + 1) * 512], lhsT=wl[k], rhs=rl[k][:, tb * 512:(tb + 1) * 512],
                                            start=(k == 0), stop=(k == n - 1))
                    return last
                P.op("pe", fn, reads=[rnames[k]] + resA + resB, writes=wres)

        def act(out, in_, func, reads, writes, **kw):
            P.op("act", lambda e: e.activation(out=out, in_=in_, func=func, **kw), reads=reads, writes=writes)

        def tt(out, in0, in1, op, reads, writes, eng="dve"):
            P.op(eng, lambda e: e.tensor_tensor(out=out, in0=in0, in1=in1, op=op), reads=reads, writes=writes)

        def stt(out, in0, scalar, in1, op0, op1, reads, writes):
            P.op("dve", lambda e: e.scalar_tensor_tensor(out=out, in0=in0, scalar=scalar, in1=in1, op0=op0, op1=op1),
                 reads=reads, writes=writes)

        def ts(out, in0, s1, s2, op0, op1, reads, writes, eng="dve"):
            if s2 is None:
                P.op(eng, lambda e: e.tensor_scalar(out=out, in0=in0, scalar1=s1, scalar2=None, op0=op0),
                     reads=reads, writes=writes)
            else:
                P.op(eng, lambda e: e.tensor_scalar(out=out, in0=in0, scalar1=s1, scalar2=s2, op0=op0, op1=op1),
                     reads=reads, writes=writes)

        def cp(eng, out, in_, reads, writes):
            if eng == "act":
                P.op("act", lambda e: e.copy(out=out, in_=in_), reads=reads, writes=writes)
            else:
                P.op(eng, lambda e: e.tensor_copy(out=out, in_=in_), reads=reads, writes=writes)

        def mset(eng, ap, val, writes):
            P.op(eng, lambda e: e.memset(ap, val), writes=writes)

        P.dma("sp", "vecs", lambda e: [e.dma_start(out=vecs[:, :], in_=vecs_in)], writes=["vecs"])
        cp("dve", identb[:, :], ident, ["vecs"], ["identb"])
        mset("dve", onesb[:, :], 1.0, ["onesb"])
        mset("dve", onesf[:, :], 1.0, ["onesf"])
        mset("dve", rmask[:, :], 1.0, ["rmask"])
        mset("dve", rmask[:, :].rearrange("p (c t) -> p c t", t=64)[:, :, 0:1], 0.0, ["rmask"])
        mset("dve", Sst[:, :, :], 0.0, [f"S{h}" for h in range(8)])
        mset("dve", ccar[:, :, :], 0.0, ["ccar"])
        mset("dve", pcarb[:, :, :], 0.0, ["pcarb"])
        lbt = vecs[:, V_LB:V_LB + 24].rearrange("p (r c) -> p r c", r=3)
        act(lbv[:, :, :], lbt, AF.Exp, ["vecs"], ["lbv"])
        tt(lbd[:, 0, :], lbv[:, 0, :], lbv[:, 1, :], ALU.add, ["lbv"], ["lbd"])
        tt(lbd[:, 0, :], lbd[:, 0, :], lbv[:, 2, :], ALU.add, ["lbv", "lbd"], ["lbd"])
        P.op("dve", lambda e: e.reciprocal(out=lbd[:, 1, :], in_=lbd[:, 0, :]), reads=["lbd"], writes=["lbd"])
        tt(lbd[:, 1, :], lbd[:, 1, :], lbv[:, 0, :], ALU.mult, ["lbv", "lbd"], ["lbd"])
        ts(lbd[:, 3, :], lbd[:, 1, :], 1.0, None, ALU.subtract, None, ["lbd"], ["lbd"])
        ts(lbd[:, 2, :], lbd[:, 3, :], -1.0, None, ALU.mult, None, ["lbd"], ["lbd"])
        for ex in range(NEXP):
            cp("dve", selc[:, ex, :], ident[0:8, ex:ex + 1].to_broadcast([8, 128]), ["vecs"], ["selc"])
        wr = vecs[:, V_WR:V_WR + 128].rearrange("p (c e) -> p c e", c=KC)
        for c in range(KC):
            ts(wrg[:, c, :], wr[:, c, :], vecs[:, V_FNO + c:V_FNO + c + 1], None, ALU.mult, None, ["vecs"], ["wrg"])

        def rms_sq(c):
            act(S16(13, c % 2), xs[:, c, :], AF.Square, [f"x{c}"], R(13, c % 2))

        def rms_mm(c):
            ps = PS(3)
            sq = S16(13, c % 2)

            def fn(e):
                last = None
                for tb in range(2):
                    last = e.matmul(ps[:, tb * 512:(tb + 1) * 512], lhsT=onesb[:, :],
                                    rhs=sq[:, tb * 512:(tb + 1) * 512], start=(c == 0), stop=(c == KC - 1))
                return last
            P.op("pe", fn, reads=R(13, c % 2) + ["onesb"], writes=RPS(3))

        def rms_accum(c):
            rms_sq(c)
            rms_mm(c)

        def rms_finish(need_sbuf=False):
            act(rstd[:, :], PS(3), AF.Ln, RPS(3), ["rstd"], scale=1.0 / D, bias=EPS)
            act(PS(3), rstd[:, :], AF.Exp, ["rstd"], RPS(3), scale=-0.5)
            if need_sbuf:
                act(rstd[:, :], rstd[:, :], AF.Exp, ["rstd"], ["rstd"], scale=-0.5)

        pend = []

        def rms_later(m, lag):
            pend.append(m)
            if len(pend) > lag:
                rms_accum(pend.pop(0))

        def rms_flush():
            while pend:
                rms_accum(pend.pop(0))

        def rms_stats(hf):
            for c in range(KC):
                rms_accum(c)
            rms_finish()

        def norm_to_h(gain_off):
            for c in range(KC):
                stt(hs[:, c, :], xs[:, c, :], vecs[:, gain_off + c:gain_off + c + 1], PS(3), ALU.mult, ALU.mult,
                    [f"x{c}", "vecs"] + RPS(3), [f"h{c}"])

        def x_add_ps(m, ps, rps):
            tt(xs[:, m, :], ps, xs[:, m, :], ALU.add, rps + [f"x{m}"], [f"x{m}"])

        pscnt = {"n": 0}

        def next_ps():
            i = pscnt["n"] % 3
            pscnt["n"] += 1
            return i

        def load_x(hf):
            for ti in range(NTILE):
                sl = 4 + 2 * (ti % 4)
                stg = scr[:, sl * 1024:(sl + 2) * 1024]
                rs = R(sl) + R(sl + 1)
                t0 = hf * T + ti * 128
                P.dma("sp", f"xin{ti % 4}", lambda e, stg=stg, t0=t0: [e.dma_start(out=stg, in_=x_in[t0:t0 + 128, :])],
                      writes=rs + (["xgate"] if ti == NTILE - 1 else []))
                for q in range(4):
                    b = next_ps()
                    bank = 2 * b

                    def fn(e, q=q, bank=bank, stg=stg):
                        last = None
                        for j in range(4):
                            c = q * 4 + j
                            last = e.transpose(psum[:, bank, j * 128:(j + 1) * 128], stg[:, c * 128:(c + 1) * 128], ident)
                        return last
                    P.op("pe", fn, reads=rs + ["vecs"], writes=[f"pb{bank}"])
                    outv = xs[:, q * 4:(q + 1) * 4, ti * 128:(ti + 1) * 128]
                    inv = psum[:, bank, :].rearrange("p (j t) -> p j t", j=4)
                    cp("act" if q % 2 == 0 else "dve", outv, inv, [f"pb{bank}"], [f"x{c}" for c in range(q * 4, q * 4 + 4)])

        def l0_mixer(hf):
            rms_stats(hf)
            norm_to_h(V_MNE)
            A, B, C, Dd, E = 4, 5, 6, 7, 8
            sring = {"n": 0}
            tail_q = []
            fwd = {}
            for hp in range(4):
                for hl in range(2):
                    hd = hp * 2 + hl
                    lo = 0
                    if hd not in fwd:
                        fwd[hd] = wcols(w_in, 0, D, 1024 + hd * 128, 128, after=["xgate"])
                    wf, rwf = fwd[hd]
                    wq, rwq = wcols(w_in, 0, D, 0 + hd * 128, 128)
                    wi, rwi = wcols(w_in, 0, D, 2048 + hd * 128, 128)
                    wg, rwg = wcols(w_in, 0, D, 3072 + hd * 128, 128)
                    oml = lbd[:, 2, hd:hd + 1]
                    noml = lbd[:, 3, hd:hd + 1]
                    if hd == 0:
                        mm_pair_kpipe(PS(0), [wf[:, k, lo:lo + 128] for k in range(KC)], rwf,
                                      PS(1), [wq[:, k, lo:lo + 128] for k in range(KC)], rwq,
                                      [hs[:, k, :] for k in range(KC)], HR, RPS(0) + RPS(1))
                        act(S32(A), PS(0), AF.Sigmoid, RPS(0), R(A), scale=-1.0)
                    else:
                        act(S32(A), PS(0), AF.Sigmoid, RPS(0), R(A), scale=-1.0)
                        if tail_q:
                            tail_q.pop(0)()
                        mm_group(PS(1), [wq[:, k, lo:lo + 128] for k in range(KC)], [hs[:, k, :] for k in range(KC)],
                                 HR + rwq, RPS(1))
                    mm_group(PS(2), [wi[:, k, lo:lo + 128] for k in range(KC)], [hs[:, k, :] for k in range(KC)],
                             HR + rwi, RPS(2))
                    mm_group(PS(0), [wg[:, k, lo:lo + 128] for k in range(KC)], [hs[:, k, :] for k in range(KC)],
                             HR + rwg, RPS(0))
                    act(S32(B), S32(A), AF.Ln, R(A) + ["lbd"], R(B), scale=noml, bias=1.0)
                    P.op("dve", lambda e: e.tensor_tensor_scan(out=S32(C), data0=rmask[:, :], data1=S32(B), initial=0.0,
                                                               op0=ALU.mult, op1=ALU.add),
                         reads=R(B) + ["rmask"], writes=R(C))
                    c3 = S32(C).rearrange("p (c t) -> p c t", t=64)
                    tt(S32(B).rearrange("p (c t) -> p c t", t=64), c3, c3[:, :, 32:33].to_broadcast([128, 16, 64]),
                       ALU.subtract, R(C), R(B))
                    tt(S32(Dd).rearrange("p (c t) -> p c t", t=64), c3, c3[:, :, 63:64].to_broadcast([128, 16, 64]),
                       ALU.subtract, R(C), R(Dd))
                    act(S32(E), S32(B), AF.Exp, R(B), R(E), scale=-1.0)
                    stt(S16(10, 0), S32(A), oml, S32(E), ALU.mult, ALU.mult, R(A) + R(E) + ["lbd"], R(10, 0))
                    act(S32(E), S32(Dd), AF.Exp, R(Dd), R(E), scale=-1.0)
                    stt(S16(10, 1), S32(A), oml, S32(E), ALU.mult, ALU.mult, R(A) + R(E) + ["lbd"], R(10, 1))
                    act(S32(B), S32(B), AF.Exp, R(B), R(B))
                    act(S32(C), S32(C), AF.Exp, R(C), R(C))
                    cp("act", S16(11, 0), PS(2), RPS(2), R(11, 0))
                    act(S32(A), PS(1), AF.Silu, RPS(1), R(A))
                    tt(S16(9, 0), S32(A), S32(B), ALU.mult, R(A) + R(B), R(9, 0))
                    tt(S16(9, 1), S32(A), S32(C), ALU.mult, R(A) + R(C), R(9, 1))
                    act(S16(hd // 2, hd % 2), PS(0), AF.Silu, RPS(0), R(hd // 2, hd % 2))
                    pv = psum[:, 4, :].bitcast(BF16)
                    pk = psum[:, 5, :].bitcast(BF16)

                    def fnt(e, src, dstp):
                        last = None
                        for ti in range(NTILE):
                            last = e.transpose(dstp[:, ti * 128:(ti + 1) * 128], src[:, ti * 128:(ti + 1) * 128], identb[:, :])
                        return last
                    P.op("pe", lambda e, pv=pv: fnt(e, S16(11, 0), pv), reads=R(11, 0) + ["identb"], writes=["pb4"])
                    P.op("pe", lambda e, pk=pk: fnt(e, S16(10, 1), pk), reads=R(10, 1) + ["identb"], writes=["pb5"])
                    cp("act", S16(11, 1), pv, ["pb4"], R(11, 1))
                    cp("dve", S16(12, 0), pk, ["pb5"], R(12, 0))
                    vtok, kltok = S16(11, 1), S16(12, 0)
                    kdT, qd, qc = S16(10, 0), S16(9, 0), S16(9, 1)
                    eqc = S32(C)
                    r0 = sring["n"] % 4
                    cp("act", Sb[r0][:, :], Sst[:, hd, :], [f"S{hd}"], [f"Sb{r0}"])
                    def core_pe1(ti):
                        tsl = slice(ti * 128, (ti + 1) * 128)
                        psc = psum[:, 4 + ti % 2, 0:128]
                        pkv0 = psum[:, 6, 0:128]
                        pkv1 = psum[:, 7, 0:128]
                        rsc, rkv0, rkv1 = f"pb{4 + ti % 2}", "pb6", "pb7"
                        P.op("pe", lambda e: e.matmul(psc, lhsT=kdT[:, tsl], rhs=qd[:, tsl], start=True, stop=True),
                             reads=R(10, 0) + R(9, 0), writes=[rsc])
                        P.op("pe", lambda e: e.matmul(pkv0, lhsT=kltok[0:64, tsl], rhs=vtok[0:64, tsl], start=True, stop=True),
                             reads=R(12, 0) + R(11, 1), writes=[rkv0])
                        P.op("pe", lambda e: e.matmul(pkv1, lhsT=kltok[64:128, tsl], rhs=vtok[64:128, tsl], start=True, stop=True),
                             reads=R(12, 0) + R(11, 1), writes=[rkv1])

                    def core_dve(ti, r1, r2):
                        psc = psum[:, 4 + ti % 2, 0:128]
                        pkv0 = psum[:, 6, 0:128]
                        pkv1 = psum[:, 7, 0:128]
                        rsc, rkv0, rkv1 = f"pb{4 + ti % 2}", "pb6", "pb7"
                        tt(Pm[ti % 2][:, :], psc, cmask, ALU.mult, [rsc, "vecs"], [f"Pm{ti % 2}"])
                        d0 = eqc[:, ti * 128 + 63:ti * 128 + 64]
                        d1 = eqc[:, ti * 128 + 127:ti * 128 + 128]
                        stt(Stmp[:, :], Sst[:, hd, :], d0, pkv0, ALU.mult, ALU.add, [f"S{hd}", rkv0] + R(C), ["Stmp"])
                        cp("act", Sb[r1][:, :], Stmp[:, :], ["Stmp"], [f"Sb{r1}"])
                        stt(Sst[:, hd, :], Stmp[:, :], d1, pkv1, ALU.mult, ALU.add, ["Stmp", rkv1] + R(C), [f"S{hd}"])
                        cp("act", Sb[r2][:, :], Sst[:, hd, :], [f"S{hd}"], [f"Sb{r2}"])

                    def core_pe2(ti, r0, r1):
                        tsl = slice(ti * 128, (ti + 1) * 128)
                        pso = psum[:, 2 + ti % 2, 0:128]
                        rpo = f"pb{2 + ti % 2}"

                        def fo(e):
                            e.matmul(pso, lhsT=vtok[:, tsl], rhs=Pm[ti % 2][:, :], start=True, stop=False)
                            e.matmul(pso[:, 0:64], lhsT=Sb[r0][:, :], rhs=qc[:, ti * 128:ti * 128 + 64], start=False, stop=False)
                            return e.matmul(pso[:, 64:128], lhsT=Sb[r1][:, :], rhs=qc[:, ti * 128 + 64:ti * 128 + 128],
                                            start=False, stop=True)
                        P.op("pe", fo, reads=R(11, 1) + [f"Pm{ti % 2}", f"Sb{r0}", f"Sb{r1}"] + R(9, 1), writes=[rpo])
                        cp("act", S32(13)[:, tsl], pso, [rpo], R(13))
                    wfn, rwfn = None, None
                    if hd + 1 < 8:
                        fwd[hd + 1] = wcols(w_in, 0, D, 1024 + (hd + 1) * 128, 128)
                        wfn, rwfn = fwd[hd + 1]

                    def fnext(kk, wfn=wfn, rwfn=rwfn):
                        def fn(e):
                            last = None
                            for tb in range(2):
                                last = e.matmul(PS(0)[:, tb * 512:(tb + 1) * 512], lhsT=wfn[:, kk, 0:128],
                                                rhs=hs[:, kk, tb * 512:(tb + 1) * 512], start=(kk == 0), stop=(kk == KC - 1))
                            return last
                        P.op("pe", fn, reads=[f"h{kk}"] + rwfn, writes=RPS(0))
                    core_pe1(0)
                    for ti in range(NTILE):
                        r0 = sring["n"] % 4
                        r1 = (r0 + 1) % 4
                        r2 = (r0 + 2) % 4
                        sring["n"] += 2
                        core_dve(ti, r1, r2)
                        if ti + 1 < NTILE:
                            core_pe1(ti + 1)
                        if hd + 1 < 8:
                            fnext(2 * ti)
                        core_pe2(ti, r0, r1)
                        if hd + 1 < 8:
                            fnext(2 * ti + 1)
                    act(S16(12, 1), S32(13), AF.Square, R(13), R(12, 1))

                    def head_tail(hd=hd):
                        oT = S32(13)

                        def fss(e):
                            e.matmul(PS(3)[:, 0:512], lhsT=onesb[:, :], rhs=S16(12, 1)[:, 0:512], start=True, stop=True)
                            return e.matmul(PS(3)[:, 512:1024], lhsT=onesb[:, :], rhs=S16(12, 1)[:, 512:1024], start=True, stop=True)
                        P.op("pe", fss, reads=R(12, 1) + ["onesb"], writes=RPS(3))
                        act(PS(3), PS(3), AF.Ln, RPS(3), RPS(3), scale=1.0 / 128, bias=EPS)
                        act(PS(3), PS(3), AF.Exp, RPS(3), RPS(3), scale=-0.5)
                        stt(oT, oT, vecs[:, V_HG + hd:V_HG + hd + 1], PS(3), ALU.mult, ALU.mult, R(13) + RPS(3) + ["vecs"], R(13))
                        ao = S16(hd // 2, hd % 2)
                        tt(ao, oT, ao, ALU.mult, R(13) + R(hd // 2, hd % 2), R(hd // 2, hd % 2))
                    tail_q.append(head_tail)
            tail_q.pop(0)()
            ABR = [r for s in range(4) for r in R(s)]
            ab = [S16(c // 2, c % 2) for c in range(8)]
            for mb in range(4):
                wv, rw = wcols(w_out, 0, 1024, mb * 512, 512)
                for ml in range(4):
                    m = mb * 4 + ml
                    pi = next_ps()
                    if m == 0:
                        def fa(e, pi=pi, wv=wv):
                            last = None
                            for k in range(7):
                                for tb in range(2):
                                    last = e.matmul(PS(pi)[:, tb * 512:(tb + 1) * 512], lhsT=wv[:, k, 0:128],
                                                    rhs=ab[k][:, tb * 512:(tb + 1) * 512], start=(k == 0), stop=False)
                            return last

                        def fb(e, pi=pi, wv=wv):
                            last = None
                            for tb in range(2):
                                last = e.matmul(PS(pi)[:, tb * 512:(tb + 1) * 512], lhsT=wv[:, 7, 0:128],
                                                rhs=ab[7][:, tb * 512:(tb + 1) * 512], start=False, stop=True)
                            return last
                        P.op("pe", fa, reads=[r for c in range(7) for r in R(c // 2, c % 2)] + rw, writes=RPS(pi))
                        P.op("pe", fb, reads=R(3, 1) + rw, writes=RPS(pi))
                    else:
                        mm_group(PS(pi), [wv[:, k, ml * 128:(ml + 1) * 128] for k in range(8)], ab, ABR + rw, RPS(pi))
                    x_add_ps(m, PS(pi), RPS(pi))
            tbuf = scr[:, 4 * 1024:4 * 1024 + 1026]
            RT = R(4) + R(5)
            for cpair in range(4):
                for cl in range(2):
                    c = cpair * 2 + cl
                    lo = 0
                    wC, rwC = wcols(w_in, 0, D, 5120 + c * 128, 128)
                    wH, rwH = wcols(w_in, 0, D, 6144 + c * 128, 128)
                    wB, rwB = wcols(w_in, 0, D, 4096 + c * 128, 128)
                    mm_group(PS(0), [wC[:, k, lo:lo + 128] for k in range(KC)], [hs[:, k, :] for k in range(KC)], HR + rwC, RPS(0))
                    cp("act", S32(7), PS(0), RPS(0), R(7))
                    mm_group(PS(1), [wH[:, k, lo:lo + 128] for k in range(KC)], [hs[:, k, :] for k in range(KC)], HR + rwH, RPS(1))
                    cp("dve", tbuf[:, 0:2], ccar[:, c, :], ["ccar"], RT)
                    tt(tbuf[:, 2:1026], S32(7), PS(1), ALU.mult, R(7) + RPS(1), RT)
                    cw0 = vecs[:, V_CW + 0 * 8 + c:V_CW + 0 * 8 + c + 1]
                    cw1 = vecs[:, V_CW + 1 * 8 + c:V_CW + 1 * 8 + c + 1]
                    cw2 = vecs[:, V_CW + 2 * 8 + c:V_CW + 2 * 8 + c + 1]
                    ts(S32(6), tbuf[:, 2:1026], cw2, None, ALU.mult, None, RT + ["vecs"], R(6))
                    stt(S32(6), tbuf[:, 1:1025], cw1, S32(6), ALU.mult, ALU.add, RT + R(6) + ["vecs"], R(6))
                    stt(S32(6), tbuf[:, 0:1024], cw0, S32(6), ALU.mult, ALU.add, RT + R(6) + ["vecs"], R(6))
                    cp("dve", ccar[:, c, :], tbuf[:, 1024:1026], RT, ["ccar"])
                    mm_group(PS(2), [wB[:, k, lo:lo + 128] for k in range(KC)], [hs[:, k, :] for k in range(KC)], HR + rwB, RPS(2))
                    tt(S16(c // 2, c % 2), S32(6), PS(2), ALU.mult, R(6) + RPS(2), R(c // 2, c % 2))
            for mb in range(4):
                wv, rw = wcols(w_out, 1024, 1024, mb * 512, 512)
                for ml in range(4):
                    m = mb * 4 + ml
                    pi = next_ps()
                    if m == 0:
                        def fa2(e, pi=pi, wv=wv):
                            last = None
                            for k in range(7):
                                for tb in range(2):
                                    last = e.matmul(PS(pi)[:, tb * 512:(tb + 1) * 512], lhsT=wv[:, k, 0:128],
                                                    rhs=ab[k][:, tb * 512:(tb + 1) * 512], start=(k == 0), stop=False)
                            return last

                        def fb2(e, pi=pi, wv=wv):
                            last = None
                            for tb in range(2):
                                last = e.matmul(PS(pi)[:, tb * 512:(tb + 1) * 512], lhsT=wv[:, 7, 0:128],
                                                rhs=ab[7][:, tb * 512:(tb + 1) * 512], start=False, stop=True)
                            return last
                        P.op("pe", fa2, reads=[r for c in range(7) for r in R(c // 2, c % 2)] + rw, writes=RPS(pi))
                        P.op("pe", fb2, reads=R(3, 1) + rw, writes=RPS(pi))
                    else:
                        mm_group(PS(pi), [wv[:, k, ml * 128:(ml + 1) * 128] for k in range(8)], ab, ABR + rw, RPS(pi))
                    x_add_ps(m, PS(pi), RPS(pi))
                    rms_later(m, 3)
            rms_flush()

        def ffn_block(Wg2, Wu2, Wd2, c0, r0, gate_e=None, tail=False, first=False):
            blk = ffn_block.n
            ffn_block.n += 1
            aslot = 4 + 2 * (blk % 2)
            actv = scr[:, aslot * 1024:(aslot + 2) * 1024].bitcast(BF16).rearrange("p (j t) -> p j t", j=4)
            for half in range(2):
                wgv, rg = wcols(Wg2, 0, D, c0 + half * 256, 256)
                wuv, ru = wcols(Wu2, 0, D, c0 + half * 256, 256)
                for jl in range(2):
                    j = half * 2 + jl
                    lo = jl * 128
                    pa = next_ps()
                    pb = next_ps()
                    if first and j == 0:
                        mm_pair_kpipe(PS(pa), [wgv[:, k, lo:lo + 128] for k in range(KC)], rg,
                                      PS(pb), [wuv[:, k, lo:lo + 128] for k in range(KC)], ru,
                                      [hs[:, k, :] for k in range(KC)], HR, RPS(pa) + RPS(pb))
                    else:
                        mm_group(PS(pa), [wgv[:, k, lo:lo + 128] for k in range(KC)], [hs[:, k, :] for k in range(KC)], HR + rg, RPS(pa))
                        mm_group(PS(pb), [wuv[:, k, lo:lo + 128] for k in range(KC)], [hs[:, k, :] for k in range(KC)], HR + ru, RPS(pb))
                    tslot = 8 + (ffn_block.t % 2)
                    ffn_block.t += 1
                    act(S32(tslot), PS(pa), AF.Silu, RPS(pa), R(tslot))
                    ares = R(aslot + j // 2, j % 2)
                    if gate_e is not None:
                        gbc = S16(gate_e // 2, gate_e % 2)
                        tt(S32(tslot), S32(tslot), gbc, ALU.mult, R(tslot) + R(gate_e // 2, gate_e % 2), R(tslot))
                    tt(actv[:, j, :], S32(tslot), PS(pb), ALU.mult, R(tslot) + RPS(pb), ares)
            wd0, rd0 = wcols(Wd2, r0, 256, 0, D)
            wd1, rd1 = wcols(Wd2, r0 + 256, 256, 0, D)
            ares_all = R(aslot) + R(aslot + 1)
            for m in range(KC):
                pi = next_ps()
                wl = [wd0[:, 0, m * 128:(m + 1) * 128], wd0[:, 1, m * 128:(m + 1) * 128],
                      wd1[:, 0, m * 128:(m + 1) * 128], wd1[:, 1, m * 128:(m + 1) * 128]]
                if m == 0:
                    def fa(e, pi=pi, wl=wl):
                        last = None
                        for j in range(3):
                            for tb in range(2):
                                last = e.matmul(PS(pi)[:, tb * 512:(tb + 1) * 512], lhsT=wl[j], rhs=actv[:, j, tb * 512:(tb + 1) * 512],
                                                start=(j == 0), stop=False)
                        return last

                    def fb(e, pi=pi, wl=wl):
                        last = None
                        for tb in range(2):
                            last = e.matmul(PS(pi)[:, tb * 512:(tb + 1) * 512], lhsT=wl[3], rhs=actv[:, 3, tb * 512:(tb + 1) * 512],
                                            start=False, stop=True)
                        return last
                    P.op("pe", fa, reads=R(aslot) + R(aslot + 1, 0) + rd0 + rd1, writes=RPS(pi))
                    P.op("pe", fb, reads=R(aslot + 1, 1) + rd1, writes=RPS(pi))
                else:
                    mm_group(PS(pi), wl, [actv[:, j, :] for j in range(4)], ares_all + rd0 + rd1, RPS(pi))
                x_add_ps(m, PS(pi), RPS(pi))
                if tail:
                    rms_later(m, 3)
            if tail:
                rms_flush()
        ffn_block.n = 0
        ffn_block.t = 0

        def l0_ffn(hf):
            rms_finish()
            norm_to_h(V_FNE)
            nb = DFF // 512
            for blk in range(nb):
                ffn_block(w_gate, w_up, w_down, blk * 512, blk * 512, tail=(blk == nb - 1), first=(blk == 0))

        def ple(hf, layer):
            rms_finish()
            norm_to_h(V_PLN + layer * 16)
            pT = S16(10).rearrange("p (f t) -> p f t", f=2)
            stg = scr[:, 11 * 1024:13 * 1024].rearrange("p (i f) -> p i f", i=NTILE)
            rs = R(11) + R(12)
            src = p_in[layer, hf * T:(hf + 1) * T, :].rearrange("(i p) f -> p i f", p=128)
            P.dma("sp", "pin", lambda e: [e.dma_start(out=stg, in_=src)], writes=rs)
            for f in range(2):
                pi = next_ps()

                def fn(e, f=f, pi=pi):
                    last = None
                    for ti in range(NTILE):
                        last = e.transpose(PS(pi)[:, ti * 128:(ti + 1) * 128], stg[:, ti, f * 128:(f + 1) * 128], ident)
                    return last
                P.op("pe", fn, reads=rs + ["vecs"], writes=RPS(pi))
                cp("act", pT[:, f, :], PS(pi), RPS(pi), R(10, f))
            wpj = scr[:, 0:2048].bitcast(BF16).rearrange("p (k n) -> p k n", k=2)
            rpjl = R(0) + R(1)
            srcpj = ple_pj[layer].rearrange("(k p) n -> p k n", p=128)
            P.dma("pool", "wpj", lambda e: [e.dma_start(out=wpj, in_=srcpj)], writes=rpjl)
            def ple_evac(m, pa, pb):
                tslot = 8 + (m % 2)
                act(S32(tslot), PS(pa), AF.Sigmoid, RPS(pa), R(tslot))
                tt(S32(tslot), S32(tslot), PS(pb), ALU.mult, R(tslot) + RPS(pb), R(tslot))
                tt(xs[:, m, :], xs[:, m, :], S32(tslot), ALU.add, R(tslot) + [f"x{m}"], [f"x{m}"])
                pend.append(m)

            def ple_pp(m, pb):
                mm_group(PS(pb), [wpj[:, f, m * 128:(m + 1) * 128] for f in range(2)], [pT[:, f, :] for f in range(2)],
                         R(10) + rpjl, RPS(pb))
            for mb in range(8):
                wv, rw = wcols(ple_gw[layer], 0, D, mb * 256, 256)
                if mb == 0:
                    pa0, pa1 = next_ps(), next_ps()
                    mm_pair_kpipe(PS(pa0), [wv[:, k, 0:128] for k in range(KC)], rw,
                                  PS(pa1), [wv[:, k, 128:256] for k in range(KC)], rw,
                                  [hs[:, k, :] for k in range(KC)], HR, RPS(pa0) + RPS(pa1))
                    pb0 = next_ps()
                    ple_pp(0, pb0)
                    ple_evac(0, pa0, pb0)
                    pb1 = next_ps()
                    ple_pp(1, pb1)
                    ple_evac(1, pa1, pb1)
                    continue
                for ml in range(2):
                    m = mb * 2 + ml
                    cur = None
                    if len(pend) >= 2:
                        cur = pend.pop(0)
                        rms_sq(cur)
                    pa = next_ps()
                    pb = next_ps()
                    mm_group(PS(pa), [wv[:, k, ml * 128:(ml + 1) * 128] for k in range(KC)], [hs[:, k, :] for k in range(KC)],
                             HR + rw, RPS(pa))
                    ple_pp(m, pb)
                    ple_evac(m, pa, pb)
                    if cur is not None:
                        rms_mm(cur)
            rms_flush()

        def l1_pool(hf):
            rms_finish()
            def stage_a(c):
                w = 2 << (c // 4)
                i = c % 2
                hnf = S32(4 + i)
                hb = S16(6 + i)[:, 0:1040]
                rf, rb = R(4 + i), R(6 + i)
                cp("act", hb[:, 0:16], pcarb[:, c, :], ["pcarb"], rb)
                stt(hnf, xs[:, c, :], vecs[:, V_MNO + c:V_MNO + c + 1], PS(3), ALU.mult, ALU.mult,
                    [f"x{c}", "vecs"] + RPS(3), rf)
                cp("act", hb[:, 16:1040], hnf, rf, rb)
                cp("act", pcarb[:, c, :], hb[:, 1024:1040], rb, ["pcarb"])
                pi = next_ps()

                def fw(e, pi=pi, w=w, hb=hb):
                    last = None
                    for tb in range(2):
                        for k in range(w):
                            o = 16 + tb * 512 - k
                            last = e.matmul(PS(pi)[:, tb * 512:(tb + 1) * 512], lhsT=identb[:, :], rhs=hb[:, o:o + 512],
                                            start=(k == 0), stop=(k == w - 1))
                    return last
                P.op("pe", fw, reads=rb + ["identb"], writes=RPS(pi))
                return pi

            def stage_b(c, pi):
                g = c // 4
                w = 2 << g
                i = c % 2
                hnf = S32(4 + i)
                rf = R(4 + i)
                stt(hs[:, c, :], PS(pi), 1.0 / w, hnf, ALU.mult, ALU.subtract, RPS(pi) + rf, [f"h{c}"])
                if hf == 0:
                    nfix = w - 1
                    fx = S32(10)[:, 0:16]
                    tt(fx[:, 0:nfix], PS(pi)[:, 0:nfix], vecs[:, V_RF + g * 16:V_RF + g * 16 + nfix], ALU.mult,
                       RPS(pi) + ["vecs"], R(10, 0))
                    tt(hs[:, c, 0:nfix], fx[:, 0:nfix], hnf[:, 0:nfix], ALU.subtract, R(10, 0) + rf, [f"h{c}"])
            pis = {0: stage_a(0)}
            for c in range(KC):
                if c + 1 < KC:
                    pis[c + 1] = stage_a(c + 1)
                stage_b(c, pis[c])
            for g in range(4):
                wv, rw = wcols(pool_w[g], 0, 512, 0, 512)
                for ml in range(4):
                    m = g * 4 + ml
                    pi = next_ps()
                    mm_group(PS(pi), [wv[:, k, ml * 128:(ml + 1) * 128] for k in range(4)], [hs[:, g * 4 + k, :] for k in range(4)],
                             [f"h{g * 4 + k}" for k in range(4)] + rw, RPS(pi))
                    stt(xs[:, m, :], PS(pi), vecs[:, V_PS + m:V_PS + m + 1], xs[:, m, :], ALU.mult, ALU.add,
                        RPS(pi) + [f"x{m}", "vecs"], [f"x{m}"])
                    rms_later(m, 3)
            rms_flush()

        def l1_moe(hf):
            rms_finish(need_sbuf=True)
            norm_to_h(V_FNO)
            XR = [f"x{c}" for c in range(KC)]

            def ftr(e):
                last = None
                for ti in range(NTILE):
                    last = e.transpose(PS(2)[:, ti * 128:(ti + 1) * 128], rstd[:, ti * 128:(ti + 1) * 128], ident)
                return last
            P.op("pe", ftr, reads=["rstd", "vecs"], writes=RPS(2))
            cp("act", rtk[:, :], PS(2).rearrange("p (i c) -> p i c", c=128)[:, :, 0], RPS(2), ["rtk"])

            def fl(e):
                last = None
                for ti in range(NTILE):
                    for c in range(KC):
                        last = e.matmul(psum[:, 2, ti * 8:(ti + 1) * 8], lhsT=xs[:, c, ti * 128:(ti + 1) * 128], rhs=wrg[:, c, :],
                                        start=(c == 0), stop=(c == KC - 1))
                return last
            P.op("pe", fl, reads=XR + ["wrg"], writes=["pb2"])
            tt(lgt[:, :, :], psum[:, 2, 0:64].rearrange("p (i e) -> p i e", e=8),
               rtk[:, :].unsqueeze(2).to_broadcast([128, NTILE, 8]), ALU.mult, ["pb2", "rtk"], ["lgt"])
            L = lgt[:, :, :]
            eq1, l2, eq2, g1, g2, gates = (gsm[:, i, :, :] for i in range(6))
            m1, m2, w1, w2 = (gm[:, i, :] for i in range(4))

            def bc(v):
                return v.unsqueeze(2).to_broadcast([128, NTILE, 8])
            P.op("dve", lambda e: e.tensor_reduce(out=m1, in_=L, axis=AX.X, op=ALU.max), reads=["lgt"], writes=["gm"])
            tt(eq1, L, bc(m1), ALU.is_equal, ["lgt", "gm"], ["gsm"])
            stt(l2, eq1, -1e30, L, ALU.mult, ALU.add, ["gsm", "lgt"], ["gsm"])
            P.op("dve", lambda e: e.tensor_reduce(out=m2, in_=l2, axis=AX.X, op=ALU.max), reads=["gsm"], writes=["gm"])
            tt(eq2, l2, bc(m2), ALU.is_equal, ["gsm", "gm"], ["gsm"])
            tt(w2, m2, m1, ALU.subtract, ["gm"], ["gm"])
            act(w1, w2, AF.Sigmoid, ["gm"], ["gm"], scale=-1.0)
            act(w2, w2, AF.Sigmoid, ["gm"], ["gm"])
            tt(g1, eq1, bc(w1), ALU.mult, ["gsm", "gm"], ["gsm"])
            tt(g2, eq2, bc(w2), ALU.mult, ["gsm", "gm"], ["gsm"])
            tt(gates, g1, g2, ALU.add, ["gsm"], ["gsm"])
            pg = next_ps()

            def fgt(e):
                last = None
                for ti in range(NTILE):
                    last = e.transpose(PS(pg)[0:8, ti * 128:(ti + 1) * 128], gsm[:, 5, ti, :], ident)
                return last
            P.op("pe", fgt, reads=["gsm", "vecs"], writes=RPS(pg))
            gT = S16(10, 0)[0:8, :]
            cp("act", gT, PS(pg)[0:8, :], RPS(pg), R(10, 0))
            for ex in range(NEXP):
                pi = next_ps()

                def fbc(e, pi=pi, ex=ex):
                    e.matmul(PS(pi)[:, 0:512], lhsT=selc[:, ex, :], rhs=gT[:, 0:512], start=True, stop=True)
                    return e.matmul(PS(pi)[:, 512:1024], lhsT=selc[:, ex, :], rhs=gT[:, 512:1024], start=True, stop=True)
                P.op("pe", fbc, reads=R(10, 0) + ["selc"], writes=RPS(pi))
                cp("act", S16(ex // 2, ex % 2), PS(pi), RPS(pi), R(ex // 2, ex % 2))
            for ex in range(NEXP):
                for blk in range(DFE // 512):
                    ffn_block(w_ge[ex], w_ue[ex], w_de[ex], blk * 512, blk * 512, gate_e=ex,
                              tail=(ex == NEXP - 1 and blk == DFE // 512 - 1))

        def store_out(hf, final):
            if final:
                rms_finish()
                for c in range(KC):
                    stt(xs[:, c, :], xs[:, c, :], vecs[:, V_FNG + c:V_FNG + c + 1], PS(3), ALU.mult, ALU.mult,
                        [f"x{c}", "vecs"] + RPS(3), [f"x{c}"])
            for ti in range(NTILE):
                sl = 4 + 2 * (ti % 4)
                stg = scr[:, sl * 1024:(sl + 2) * 1024]
                rs = R(sl) + R(sl + 1)
                for q in range(4):
                    b = next_ps()
                    bank = 2 * b

                    def fn(e, q=q, bank=bank, ti=ti):
                        last = None
                        for j in range(4):
                            c = q * 4 + j
                            last = e.transpose(psum[:, bank, j * 128:(j + 1) * 128], xs[:, c, ti * 128:(ti + 1) * 128], ident)
                        return last
                    P.op("pe", fn, reads=[f"x{c}" for c in range(q * 4, q * 4 + 4)] + ["vecs"], writes=[f"pb{bank}"])
                    cp("act", stg[:, q * 512:(q + 1) * 512], psum[:, bank, :], [f"pb{bank}"], [rs[q]])
                t0 = hf * T + ti * 128
                P.dma("sp", f"yout{ti % 4}", lambda e, stg=stg, t0=t0: [e.dma_start(out=y_out[t0:t0 + 128, :], in_=stg)],
                      reads=rs, writes=[f"y{hf}_{ti}"])

        for hf in range(n_halves):
            load_x(hf)
            if stop_after >= 1:
                l0_mixer(hf)
            if stop_after >= 2:
                l0_ffn(hf)
            if stop_after >= 3:
                ple(hf, 0)
            if stop_after >= 4:
                l1_pool(hf)
            if stop_after >= 5:
                l1_moe(hf)
            if stop_after >= 6:
                ple(hf, 1)
            store_out(hf, final=(do_final and stop_after >= 6))
        finals = [P.last_w[f"y{hf}_{ti}"] for hf in range(n_halves) for ti in range(NTILE)]
        fin = {}
        for k, v in finals:
            fin[k] = max(fin.get(k, 0), v)
        P.final_wait("sp", list(fin.items()))

        sems = {}
        for k in P.semkeys:
            sems[k] = es.enter_context(nc.semaphore(k.replace(":", "_")))
        with nc.Block() as block:
            P.emit(block, sems)
    return nc


def pack_vecs(inp):
    v = np.zeros((128, NV), np.float32)

    def pc(a):
        a = np.asarray(a, np.float32).reshape(-1, 128)
        return a.T
    v[:, V_FNG:V_FNG + 16] = pc(inp["final_norm_gain"])
    v[:, V_MNE:V_MNE + 16] = pc(inp["mix_norm_even"][0])
    v[:, V_HG:V_HG + 8] = pc(inp["hgrn_norm_gain"][0])
    for j in range(3):
        v[:, V_CW + j * 8:V_CW + j * 8 + 8] = pc(inp["conv_w"][0, j])
    v[:, V_FNE:V_FNE + 16] = pc(inp["ffn_norm_even"][0])
    v[:, V_MNO:V_MNO + 16] = pc(inp["mix_norm_odd"][0])
    v[:, V_PS:V_PS + 16] = pc(inp["pool_scale"][0])
    v[:, V_FNO:V_FNO + 16] = pc(inp["ffn_norm_odd"][0])
    for l in range(2):
        v[:, V_PLN + l * 16:V_PLN + l * 16 + 16] = pc(inp["ple_norm"][l])
    for r in range(3):
        v[:, V_LB + r * 8:V_LB + r * 8 + 8] = pc(inp["lb_table"][r])
    wr = np.asarray(inp["w_router"][0], np.float32).reshape(16, 128, 8)
    v[:, V_WR:V_WR + 128] = wr.transpose(1, 0, 2).reshape(128, 128)
    v[:, V_ID:V_ID + 128] = np.eye(128, dtype=np.float32)
    s = np.arange(128)[:, None]
    t = np.arange(128)[None, :]
    v[:, V_CM:V_CM + 128] = ((s // 64 == t // 64) & (s <= t)).astype(np.float32)
    for g in range(4):
        w = 2 << g
        v[:, V_RF + g * 16:V_RF + g * 16 + 16] = (1.0 / np.minimum(np.arange(1, 17), w)).astype(np.float32)[None, :]
    return v


_NC_CACHE = {}


def make_in_maps(inp):
    f = lambda a: np.ascontiguousarray(np.asarray(a, dtype=np.float32))
    vecs = pack_vecs(inp)
    shared = dict(
        vecs=vecs,
        w_in=f(inp["w_in_even"][0]), w_out=f(inp["w_out_even"][0]),
        w_gate=f(inp["w_gate_dense"][0]), w_up=f(inp["w_up_dense"][0]), w_down=f(inp["w_down_dense"][0]),
        pool_w=f(inp["pool_w"][0]), w_ge=f(inp["w_gate_exp"][0]), w_ue=f(inp["w_up_exp"][0]), w_de=f(inp["w_down_exp"][0]),
        ple_gw=f(inp["ple_gate_w"]), ple_pj=f(inp["ple_proj"]),
    )
    x = np.asarray(inp["x"], np.float32)
    p = np.asarray(inp["p"], np.float32)
    maps = []
    for b in range(8):
        m = dict(shared)
        m["x_in"] = f(x[b])
        m["p_in"] = f(p[:, b])
        maps.append(m)
    return maps


def kernel(**inputs):
    key = "full"
    if key not in _NC_CACHE:
        _NC_CACHE[key] = build()
    nc = _NC_CACHE[key]
    maps = make_in_maps(inputs)
    res = run_bass_kernel_spmd(nc, maps, core_ids=list(range(8)))
    out = np.stack([np.asarray(res.results[b]["y_out"], dtype=np.float32) for b in range(8)], axis=0)
    return out
```

```python
import numpy as np
from contextlib import ExitStack
import concourse.bass as bass
import concourse.mybir as mybir
from concourse.bass_utils import run_bass_kernel_spmd

F32 = mybir.dt.float32
BF16 = mybir.dt.bfloat16
AF = mybir.ActivationFunctionType
ALU = mybir.AluOpType
AX = mybir.AxisListType

D = 2048
KC = 16
S_FULL = 2048
T = 1024
NTILE = T // 128
EPS = 1e-6
DFF = 5632
NEXP = 8
DFE = 1024

V_FNG, V_MNE, V_HG, V_CW, V_FNE, V_MNO, V_PS, V_FNO, V_PLN, V_LB, V_WR = 0, 16, 32, 40, 64, 80, 96, 112, 128, 160, 184
V_ID, V_CM, V_RF = 312, 440, 568
NV = 632

ENGS = ("pe", "act", "dve", "pool", "sp")


class Prog:
    def __init__(self):
        self.ops = {e: [] for e in ENGS}
        self.count = {}
        self.last_w = {}
        self.readers = {}
        self.waited = {e: {} for e in ENGS}
        self.semkeys = list(ENGS)

    def _deps(self, reads, writes):
        need = {}

        def add(k, v):
            if need.get(k, 0) < v:
                need[k] = v
        for r in reads:
            d = self.last_w.get(r)
            if d is not None:
                add(*d)
        for w in writes:
            d = self.last_w.get(w)
            if d is not None:
                add(*d)
            for k, v in self.readers.get(w, {}).items():
                add(k, v)
        return need

    def _commit(self, reads, writes, me):
        k, v = me
        for r in reads:
            rd = self.readers.setdefault(r, {})
            if rd.get(k, 0) < v:
                rd[k] = v
        for w in writes:
            self.last_w[w] = me
            self.readers[w] = {}

    def _filter(self, eng, need):
        waits = []
        for k, v in need.items():
            if k == "pe" and eng == "pe":
                continue
            if self.waited[eng].get(k, 0) >= v:
                continue
            self.waited[eng][k] = v
            waits.append((k, v))
        return waits

    def op(self, eng, fn, reads=(), writes=()):
        need = self._deps(reads, writes)
        waits = self._filter(eng, need)
        self.count[eng] = self.count.get(eng, 0) + 1
        me = (eng, self.count[eng])
        self._commit(reads, writes, me)
        self.ops[eng].append((waits, fn, (eng, 1)))
        return me

    def dma(self, eng, slot, fn, reads=(), writes=(), n=1):
        key = "dma:" + slot
        if key not in self.semkeys:
            self.semkeys.append(key)
        need = self._deps(reads, writes)
        waits = self._filter(eng, need)
        self.count[key] = self.count.get(key, 0) + 16 * n
        me = (key, self.count[key])
        self._commit(reads, writes, me)
        self.ops[eng].append((waits, fn, (key, 16)))
        return me

    def final_wait(self, eng, deps):
        self.ops[eng].append((list(deps), None, None))

    def emit(self, block, sems):
        def run(engobj, name):
            for waits, fn, inc in self.ops[name]:
                for k, v in waits:
                    engobj.wait_ge(sems[k], v)
                if fn is None:
                    continue
                r = fn(engobj)
                if isinstance(r, (list, tuple)):
                    for ins in r:
                        ins.then_inc(sems[inc[0]], inc[1])
                else:
                    r.then_inc(sems[inc[0]], inc[1])

        @block.tensor
        def _(e):
            run(e, "pe")

        @block.scalar
        def _(e):
            run(e, "act")

        @block.vector
        def _(e):
            run(e, "dve")

        @block.gpsimd
        def _(e):
            run(e, "pool")

        @block.sync
        def _(e):
            run(e, "sp")


def build(stop_after=6, n_halves=2, do_final=True):
    nc = bass.Bass("TRN2", target_bir_lowering=False)

    def din(name, shape):
        return nc.dram_tensor(name, list(shape), F32, kind="ExternalInput").ap()
    x_in = din("x_in", [S_FULL, D])
    p_in = din("p_in", [2, S_FULL, 256])
    vecs_in = din("vecs", [128, NV])
    w_in = din("w_in", [D, 7168])
    w_out = din("w_out", [D, D])
    w_gate = din("w_gate", [D, DFF])
    w_up = din("w_up", [D, DFF])
    w_down = din("w_down", [DFF, D])
    pool_w = din("pool_w", [4, 512, 512])
    w_ge = din("w_ge", [NEXP, D, DFE])
    w_ue = din("w_ue", [NEXP, D, DFE])
    w_de = din("w_de", [NEXP, DFE, D])
    ple_gw = din("ple_gw", [2, D, D])
    ple_pj = din("ple_pj", [2, 256, D])
    y_out = nc.dram_tensor("y_out", [S_FULL, D], F32, kind="ExternalOutput").ap()

    es = ExitStack()
    with es:
        def sb(name, shape, dt):
            return es.enter_context(nc.sbuf_tensor(name, list(shape), dt))
        xs = sb("xs", [128, KC, T], F32)
        hs = sb("hs", [128, KC, T], BF16)
        NWH = 8
        wb = sb("wb", [128, NWH * 2048], BF16)
        NSCR = 14
        scr = sb("scr", [128, NSCR * 1024], F32)
        rstd = sb("rstd", [128, T], F32)
        vecs = sb("vecs_sb", [128, NV], F32)
        Sst = sb("Sst", [128, 8, 128], F32)
        Sb = [sb(f"Sb{i}", [128, 128], BF16) for i in range(4)]
        Stmp = sb("Stmp", [128, 128], F32)
        Pm = [sb(f"Pm{i}", [128, 128], BF16) for i in range(2)]
        identb = sb("identb", [128, 128], BF16)
        onesb = sb("onesb", [128, 128], BF16)
        onesf = sb("onesf", [128, 128], F32)
        rmask = sb("rmask", [128, T], F32)
        lbv = sb("lbv", [128, 3, 8], F32)
        lbd = sb("lbd", [128, 4, 8], F32)
        ccar = sb("ccar", [128, 8, 2], F32)
        pcarb = sb("pcarb", [128, KC, 16], BF16)
        wrg = sb("wrg", [128, KC, 8], F32)
        lgt = sb("lgt", [128, NTILE, 8], F32)
        gsm = sb("gsm", [128, 6, NTILE, 8], F32)
        gm = sb("gm", [128, 4, NTILE], F32)
        rtk = sb("rtk", [128, NTILE], F32)
        selc = sb("selc", [8, NEXP, 128], BF16)
        psum = es.enter_context(nc.psum_tensor("psum", [128, 8, 512], F32))
        psflat = psum[:, :, :].rearrange("p b n -> p (b n)")

        P = Prog()
        ident = vecs[:, V_ID:V_ID + 128]
        cmask = vecs[:, V_CM:V_CM + 128]

        def S32(slot, n=1024, off=0):
            return scr[:, slot * 1024 + off: slot * 1024 + off + n]

        def S16(slot, half=None):
            v = scr[:, slot * 1024:(slot + 1) * 1024].bitcast(BF16)
            if half is None:
                return v
            return v[:, half * 1024:(half + 1) * 1024]

        def R(slot, half=None):
            if half is None:
                return [f"s{slot}a", f"s{slot}b"]
            return [f"s{slot}{'ab'[half]}"]

        def PS(i):
            return psflat[:, i * 1024:(i + 1) * 1024]

        def RPS(i):
            return [f"pb{2 * i}", f"pb{2 * i + 1}"]

        HR = [f"h{c}" for c in range(KC)]
        wstate = {"n": 0}

        def wload(src3, kcn, ncols, after=()):
            size = kcn * ncols
            nhs = (size + 2047) // 2048
            assert nhs in (1, 2)
            if nhs == 2 and wstate["n"] % 2 == 1:
                wstate["n"] += 1
            idx = wstate["n"] % NWH
            wstate["n"] += nhs
            dst = wb[:, idx * 2048: idx * 2048 + size].rearrange("p (k n) -> p k n", k=kcn)
            res = [f"wh{idx + i}" for i in range(nhs)]
            P.dma("pool", f"w{idx}", lambda e: [e.dma_start(out=dst, in_=src3)], reads=list(after), writes=res)
            return dst, res

        def wcols(W2, r0, nrows, c0, ncols, after=()):
            src = W2[r0:r0 + nrows, c0:c0 + ncols].rearrange("(k p) n -> p k n", p=128)
            return wload(src, nrows // 128, ncols, after=after)

        def mm_group(ps, wl, rl, reads, wres):
            n = len(wl)

            def fn(e):
                last = None
                for i in range(n):
                    for tb in range(2):
                        last = e.matmul(ps[:, tb * 512:(tb + 1) * 512], lhsT=wl[i],
                                        rhs=rl[i][:, tb * 512:(tb + 1) * 512],
                                        start=(i == 0), stop=(i == n - 1))
                return last
            P.op("pe", fn, reads=reads, writes=wres)

        def mm_pair_kpipe(psA, wlA, resA, psB, wlB, resB, rl, rnames, wres):
            n = len(rl)
            for k in range(n):
                def fn(e, k=k):
                    last = None
                    for (ps, wl) in ((psA, wlA), (psB, wlB)):
                        for tb in range(2):
                            last = e.matmul(ps[:, tb * 512:(tb + 1) * 512], lhsT=wl[k], rhs=rl[k][:, tb * 512:(tb + 1) * 512],
                                            start=(k == 0), stop=(k == n - 1))
                    return last
                P.op("pe", fn, reads=[rnames[k]] + resA + resB, writes=wres)

        def act(out, in_, func, reads, writes, **kw):
            P.op("act", lambda e: e.activation(out=out, in_=in_, func=func, **kw), reads=reads, writes=writes)

        def tt(out, in0, in1, op, reads, writes, eng="dve"):
            P.op(eng, lambda e: e.tensor_tensor(out=out, in0=in0, in1=in1, op=op), reads=reads, writes=writes)

        def stt(out, in0, scalar, in1, op0, op1, reads, writes):
            P.op("dve", lambda e: e.scalar_tensor_tensor(out=out, in0=in0, scalar=scalar, in1=in1, op0=op0, op1=op1),
                 reads=reads, writes=writes)

        def ts(out, in0, s1, s2, op0, op1, reads, writes, eng="dve"):
            if s2 is None:
                P.op(eng, lambda e: e.tensor_scalar(out=out, in0=in0, scalar1=s1, scalar2=None, op0=op0),
                     reads=reads, writes=writes)
            else:
                P.op(eng, lambda e: e.tensor_scalar(out=out, in0=in0, scalar1=s1, scalar2=s2, op0=op0, op1=op1),
                     reads=reads, writes=writes)

        def cp(eng, out, in_, reads, writes):
            if eng == "act":
                P.op("act", lambda e: e.copy(out=out, in_=in_), reads=reads, writes=writes)
            else:
                P.op(eng, lambda e: e.tensor_copy(out=out, in_=in_), reads=reads, writes=writes)

        def mset(eng, ap, val, writes):
            P.op(eng, lambda e: e.memset(ap, val), writes=writes)

        P.dma("sp", "vecs", lambda e: [e.dma_start(out=vecs[:, :], in_=vecs_in)], writes=["vecs"])
        cp("dve", identb[:, :], ident, ["vecs"], ["identb"])
        mset("dve", onesb[:, :], 1.0, ["onesb"])
        mset("dve", onesf[:, :], 1.0, ["onesf"])
        mset("dve", rmask[:, :], 1.0, ["rmask"])
        mset("dve", rmask[:, :].rearrange("p (c t) -> p c t", t=64)[:, :, 0:1], 0.0, ["rmask"])
        mset("dve", Sst[:, :, :], 0.0, [f"S{h}" for h in range(8)])
        mset("dve", ccar[:, :, :], 0.0, ["ccar"])
        mset("dve", pcarb[:, :, :], 0.0, ["pcarb"])
        lbt = vecs[:, V_LB:V_LB + 24].rearrange("p (r c) -> p r c", r=3)
        act(lbv[:, :, :], lbt, AF.Exp, ["vecs"], ["lbv"])
        tt(lbd[:, 0, :], lbv[:, 0, :], lbv[:, 1, :], ALU.add, ["lbv"], ["lbd"])
        tt(lbd[:, 0, :], lbd[:, 0, :], lbv[:, 2, :], ALU.add, ["lbv", "lbd"], ["lbd"])
        P.op("dve", lambda e: e.reciprocal(out=lbd[:, 1, :], in_=lbd[:, 0, :]), reads=["lbd"], writes=["lbd"])
        tt(lbd[:, 1, :], lbd[:, 1, :], lbv[:, 0, :], ALU.mult, ["lbv", "lbd"], ["lbd"])
        ts(lbd[:, 3, :], lbd[:, 1, :], 1.0, None, ALU.subtract, None, ["lbd"], ["lbd"])
        ts(lbd[:, 2, :], lbd[:, 3, :], -1.0, None, ALU.mult, None, ["lbd"], ["lbd"])
        for ex in range(NEXP):
            cp("dve", selc[:, ex, :], ident[0:8, ex:ex + 1].to_broadcast([8, 128]), ["vecs"], ["selc"])
        wr = vecs[:, V_WR:V_WR + 128].rearrange("p (c e) -> p c e", c=KC)
        for c in range(KC):
            ts(wrg[:, c, :], wr[:, c, :], vecs[:, V_FNO + c:V_FNO + c + 1], None, ALU.mult, None, ["vecs"], ["wrg"])

        def rms_sq(c):
            act(S16(13, c % 2), xs[:, c, :], AF.Square, [f"x{c}"], R(13, c % 2))

        def rms_mm(c):
            ps = PS(3)
            sq = S16(13, c % 2)

            def fn(e):
                last = None
                for tb in range(2):
                    last = e.matmul(ps[:, tb * 512:(tb + 1) * 512], lhsT=onesb[:, :],
                                    rhs=sq[:, tb * 512:(tb + 1) * 512], start=(c == 0), stop=(c == KC - 1))
                return last
            P.op("pe", fn, reads=R(13, c % 2) + ["onesb"], writes=RPS(3))

        def rms_accum(c):
            rms_sq(c)
            rms_mm(c)

        def rms_finish(need_sbuf=False):
            act(rstd[:, :], PS(3), AF.Ln, RPS(3), ["rstd"], scale=1.0 / D, bias=EPS)
            act(PS(3), rstd[:, :], AF.Exp, ["rstd"], RPS(3), scale=-0.5)
            if need_sbuf:
                act(rstd[:, :], rstd[:, :], AF.Exp, ["rstd"], ["rstd"], scale=-0.5)

        pend = []

        def rms_later(m, lag):
            pend.append(m)
            if len(pend) > lag:
                rms_accum(pend.pop(0))

        def rms_flush():
            while pend:
                rms_accum(pend.pop(0))

        def rms_stats(hf):
            for c in range(KC):
                rms_accum(c)
            rms_finish()

        def norm_to_h(gain_off):
            for c in range(KC):
                stt(hs[:, c, :], xs[:, c, :], vecs[:, gain_off + c:gain_off + c + 1], PS(3), ALU.mult, ALU.mult,
                    [f"x{c}", "vecs"] + RPS(3), [f"h{c}"])

        def x_add_ps(m, ps, rps):
            tt(xs[:, m, :], ps, xs[:, m, :], ALU.add, rps + [f"x{m}"], [f"x{m}"])

        pscnt = {"n": 0}

        def next_ps():
            i = pscnt["n"] % 3
            pscnt["n"] += 1
            return i

        def load_x(hf):
            for ti in range(NTILE):
                sl = 4 + 2 * (ti % 4)
                stg = scr[:, sl * 1024:(sl + 2) * 1024]
                rs = R(sl) + R(sl + 1)
                t0 = hf * T + ti * 128
                P.dma("sp", f"xin{ti % 4}", lambda e, stg=stg, t0=t0: [e.dma_start(out=stg, in_=x_in[t0:t0 + 128, :])],
                      writes=rs + (["xgate"] if ti == NTILE - 1 else []))
                for q in range(4):
                    b = next_ps()
                    bank = 2 * b

                    def fn(e, q=q, bank=bank, stg=stg):
                        last = None
                        for j in range(4):
                            c = q * 4 + j
                            last = e.transpose(psum[:, bank, j * 128:(j + 1) * 128], stg[:, c * 128:(c + 1) * 128], ident)
                        return last
                    P.op("pe", fn, reads=rs + ["vecs"], writes=[f"pb{bank}"])
                    outv = xs[:, q * 4:(q + 1) * 4, ti * 128:(ti + 1) * 128]
                    inv = psum[:, bank, :].rearrange("p (j t) -> p j t", j=4)
                    cp("act" if q % 2 == 0 else "dve", outv, inv, [f"pb{bank}"], [f"x{c}" for c in range(q * 4, q * 4 + 4)])

        def l0_mixer(hf):
            rms_stats(hf)
            norm_to_h(V_MNE)
            A, B, C, Dd, E = 4, 5, 6, 7, 8
            sring = {"n": 0}
            tail_q = []
            fwd = {}
            for hp in range(4):
                for hl in range(2):
                    hd = hp * 2 + hl
                    lo = 0
                    if hd not in fwd:
                        fwd[hd] = wcols(w_in, 0, D, 1024 + hd * 128, 128, after=["xgate"])
                    wf, rwf = fwd[hd]
                    wq, rwq = wcols(w_in, 0, D, 0 + hd * 128, 128)
                    wi, rwi = wcols(w_in, 0, D, 2048 + hd * 128, 128)
                    wg, rwg = wcols(w_in, 0, D, 3072 + hd * 128, 128)
                    oml = lbd[:, 2, hd:hd + 1]
                    noml = lbd[:, 3, hd:hd + 1]
                    if hd == 0:
                        mm_pair_kpipe(PS(0), [wf[:, k, lo:lo + 128] for k in range(KC)], rwf,
                                      PS(1), [wq[:, k, lo:lo + 128] for k in range(KC)], rwq,
                                      [hs[:, k, :] for k in range(KC)], HR, RPS(0) + RPS(1))
                        act(S32(A), PS(0), AF.Sigmoid, RPS(0), R(A), scale=-1.0)
                    else:
                        act(S32(A), PS(0), AF.Sigmoid, RPS(0), R(A), scale=-1.0)
                        if tail_q:
                            tail_q.pop(0)()
                        mm_group(PS(1), [wq[:, k, lo:lo + 128] for k in range(KC)], [hs[:, k, :] for k in range(KC)],
                                 HR + rwq, RPS(1))
                    mm_group(PS(2), [wi[:, k, lo:lo + 128] for k in range(KC)], [hs[:, k, :] for k in range(KC)],
                             HR + rwi, RPS(2))
                    mm_group(PS(0), [wg[:, k, lo:lo + 128] for k in range(KC)], [hs[:, k, :] for k in range(KC)],
                             HR + rwg, RPS(0))
                    act(S32(B), S32(A), AF.Ln, R(A) + ["lbd"], R(B), scale=noml, bias=1.0)
                    P.op("dve", lambda e: e.tensor_tensor_scan(out=S32(C), data0=rmask[:, :], data1=S32(B), initial=0.0,
                                                               op0=ALU.mult, op1=ALU.add),
                         reads=R(B) + ["rmask"], writes=R(C))
                    c3 = S32(C).rearrange("p (c t) -> p c t", t=64)
                    tt(S32(B).rearrange("p (c t) -> p c t", t=64), c3, c3[:, :, 32:33].to_broadcast([128, 16, 64]),
                       ALU.subtract, R(C), R(B))
                    tt(S32(Dd).rearrange("p (c t) -> p c t", t=64), c3, c3[:, :, 63:64].to_broadcast([128, 16, 64]),
                       ALU.subtract, R(C), R(Dd))
                    act(S32(E), S32(B), AF.Exp, R(B), R(E), scale=-1.0)
                    stt(S16(10, 0), S32(A), oml, S32(E), ALU.mult, ALU.mult, R(A) + R(E) + ["lbd"], R(10, 0))
                    act(S32(E), S32(Dd), AF.Exp, R(Dd), R(E), scale=-1.0)
                    stt(S16(10, 1), S32(A), oml, S32(E), ALU.mult, ALU.mult, R(A) + R(E) + ["lbd"], R(10, 1))
                    act(S32(B), S32(B), AF.Exp, R(B), R(B))
                    act(S32(C), S32(C), AF.Exp, R(C), R(C))
                    cp("act", S16(11, 0), PS(2), RPS(2), R(11, 0))
                    act(S32(A), PS(1), AF.Silu, RPS(1), R(A))
                    tt(S16(9, 0), S32(A), S32(B), ALU.mult, R(A) + R(B), R(9, 0))
                    tt(S16(9, 1), S32(A), S32(C), ALU.mult, R(A) + R(C), R(9, 1))
                    act(S16(hd // 2, hd % 2), PS(0), AF.Silu, RPS(0), R(hd // 2, hd % 2))
                    pv = psum[:, 4, :].bitcast(BF16)
                    pk = psum[:, 5, :].bitcast(BF16)

                    def fnt(e, src, dstp):
                        last = None
                        for ti in range(NTILE):
                            last = e.transpose(dstp[:, ti * 128:(ti + 1) * 128], src[:, ti * 128:(ti + 1) * 128], identb[:, :])
                        return last
                    P.op("pe", lambda e, pv=pv: fnt(e, S16(11, 0), pv), reads=R(11, 0) + ["identb"], writes=["pb4"])
                    P.op("pe", lambda e, pk=pk: fnt(e, S16(10, 1), pk), reads=R(10, 1) + ["identb"], writes=["pb5"])
                    cp("act", S16(11, 1), pv, ["pb4"], R(11, 1))
                    cp("dve", S16(12, 0), pk, ["pb5"], R(12, 0))
                    vtok, kltok = S16(11, 1), S16(12, 0)
                    kdT, qd, qc = S16(10, 0), S16(9, 0), S16(9, 1)
                    eqc = S32(C)
                    r0 = sring["n"] % 4
                    cp("act", Sb[r0][:, :], Sst[:, hd, :], [f"S{hd}"], [f"Sb{r0}"])
                    def core_pe1(ti):
                        tsl = slice(ti * 128, (ti + 1) * 128)
                        psc = psum[:, 4 + ti % 2, 0:128]
                        pkv0 = psum[:, 6, 0:128]
                        pkv1 = psum[:, 7, 0:128]
                        rsc, rkv0, rkv1 = f"pb{4 + ti % 2}", "pb6", "pb7"
                        P.op("pe", lambda e: e.matmul(psc, lhsT=kdT[:, tsl], rhs=qd[:, tsl], start=True, stop=True),
                             reads=R(10, 0) + R(9, 0), writes=[rsc])
                        P.op("pe", lambda e: e.matmul(pkv0, lhsT=kltok[0:64, tsl], rhs=vtok[0:64, tsl], start=True, stop=True),
                             reads=R(12, 0) + R(11, 1), writes=[rkv0])
                        P.op("pe", lambda e: e.matmul(pkv1, lhsT=kltok[64:128, tsl], rhs=vtok[64:128, tsl], start=True, stop=True),
                             reads=R(12, 0) + R(11, 1), writes=[rkv1])

                    def core_dve(ti, r1, r2):
                        psc = psum[:, 4 + ti % 2, 0:128]
                        pkv0 = psum[:, 6, 0:128]
                        pkv1 = psum[:, 7, 0:128]
                        rsc, rkv0, rkv1 = f"pb{4 + ti % 2}", "pb6", "pb7"
                        tt(Pm[ti % 2][:, :], psc, cmask, ALU.mult, [rsc, "vecs"], [f"Pm{ti % 2}"])
                        d0 = eqc[:, ti * 128 + 63:ti * 128 + 64]
                        d1 = eqc[:, ti * 128 + 127:ti * 128 + 128]
                        stt(Stmp[:, :], Sst[:, hd, :], d0, pkv0, ALU.mult, ALU.add, [f"S{hd}", rkv0] + R(C), ["Stmp"])
                        cp("act", Sb[r1][:, :], Stmp[:, :], ["Stmp"], [f"Sb{r1}"])
                        stt(Sst[:, hd, :], Stmp[:, :], d1, pkv1, ALU.mult, ALU.add, ["Stmp", rkv1] + R(C), [f"S{hd}"])
                        cp("act", Sb[r2][:, :], Sst[:, hd, :], [f"S{hd}"], [f"Sb{r2}"])

                    def core_pe2(ti, r0, r1):
                        tsl = slice(ti * 128, (ti + 1) * 128)
                        pso = psum[:, 2 + ti % 2, 0:128]
                        rpo = f"pb{2 + ti % 2}"

                        def fo(e):
                            e.matmul(pso, lhsT=vtok[:, tsl], rhs=Pm[ti % 2][:, :], start=True, stop=False)
                            e.matmul(pso[:, 0:64], lhsT=Sb[r0][:, :], rhs=qc[:, ti * 128:ti * 128 + 64], start=False, stop=False)
                            return e.matmul(pso[:, 64:128], lhsT=Sb[r1][:, :], rhs=qc[:, ti * 128 + 64:ti * 128 + 128],
                                            start=False, stop=True)
                        P.op("pe", fo, reads=R(11, 1) + [f"Pm{ti % 2}", f"Sb{r0}", f"Sb{r1}"] + R(9, 1), writes=[rpo])
                        cp("act", S32(13)[:, tsl], pso, [rpo], R(13))
                    wfn, rwfn = None, None
                    if hd + 1 < 8:
                        fwd[hd + 1] = wcols(w_in, 0, D, 1024 + (hd + 1) * 128, 128)
                        wfn, rwfn = fwd[hd + 1]

                    def fnext(kk, wfn=wfn, rwfn=rwfn):
                        def fn(e):
                            last = None
                            for tb in range(2):
                                last = e.matmul(PS(0)[:, tb * 512:(tb + 1) * 512], lhsT=wfn[:, kk, 0:128],
                                                rhs=hs[:, kk, tb * 512:(tb + 1) * 512], start=(kk == 0), stop=(kk == KC - 1))
                            return last
                        P.op("pe", fn, reads=[f"h{kk}"] + rwfn, writes=RPS(0))
                    core_pe1(0)
                    for ti in range(NTILE):
                        r0 = sring["n"] % 4
                        r1 = (r0 + 1) % 4
                        r2 = (r0 + 2) % 4
                        sring["n"] += 2
                        core_dve(ti, r1, r2)
                        if ti + 1 < NTILE:
                            core_pe1(ti + 1)
                        if hd + 1 < 8:
                            fnext(2 * ti)
                        core_pe2(ti, r0, r1)
                        if hd + 1 < 8:
                            fnext(2 * ti + 1)
                    act(S16(12, 1), S32(13), AF.Square, R(13), R(12, 1))

                    def head_tail(hd=hd):
                        oT = S32(13)

                        def fss(e):
                            e.matmul(PS(3)[:, 0:512], lhsT=onesb[:, :], rhs=S16(12, 1)[:, 0:512], start=True, stop=True)
                            return e.matmul(PS(3)[:, 512:1024], lhsT=onesb[:, :], rhs=S16(12, 1)[:, 512:1024], start=True, stop=True)
                        P.op("pe", fss, reads=R(12, 1) + ["onesb"], writes=RPS(3))
                        act(PS(3), PS(3), AF.Ln, RPS(3), RPS(3), scale=1.0 / 128, bias=EPS)
                        act(PS(3), PS(3), AF.Exp, RPS(3), RPS(3), scale=-0.5)
                        stt(oT, oT, vecs[:, V_HG + hd:V_HG + hd + 1], PS(3), ALU.mult, ALU.mult, R(13) + RPS(3) + ["vecs"], R(13))
                        ao = S16(hd // 2, hd % 2)
                        tt(ao, oT, ao, ALU.mult, R(13) + R(hd // 2, hd % 2), R(hd // 2, hd % 2))
                    tail_q.append(head_tail)
            tail_q.pop(0)()
            ABR = [r for s in range(4) for r in R(s)]
            ab = [S16(c // 2, c % 2) for c in range(8)]
            for mb in range(4):
                wv, rw = wcols(w_out, 0, 1024, mb * 512, 512)
                for ml in range(4):
                    m = mb * 4 + ml
                    pi = next_ps()
                    if m == 0:
                        def fa(e, pi=pi, wv=wv):
                            last = None
                            for k in range(7):
                                for tb in range(2):
                                    last = e.matmul(PS(pi)[:, tb * 512:(tb + 1) * 512], lhsT=wv[:, k, 0:128],
                                                    rhs=ab[k][:, tb * 512:(tb + 1) * 512], start=(k == 0), stop=False)
                            return last

                        def fb(e, pi=pi, wv=wv):
                            last = None
                            for tb in range(2):
                                last = e.matmul(PS(pi)[:, tb * 512:(tb + 1) * 512], lhsT=wv[:, 7, 0:128],
                                                rhs=ab[7][:, tb * 512:(tb + 1) * 512], start=False, stop=True)
                            return last
                        P.op("pe", fa, reads=[r for c in range(7) for r in R(c // 2, c % 2)] + rw, writes=RPS(pi))
                        P.op("pe", fb, reads=R(3, 1) + rw, writes=RPS(pi))
                    else:
                        mm_group(PS(pi), [wv[:, k, ml * 128:(ml + 1) * 128] for k in range(8)], ab, ABR + rw, RPS(pi))
                    x_add_ps(m, PS(pi), RPS(pi))
            tbuf = scr[:, 4 * 1024:4 * 1024 + 1026]
            RT = R(4) + R(5)
            for cpair in range(4):
                for cl in range(2):
                    c = cpair * 2 + cl
                    lo = 0
                    wC, rwC = wcols(w_in, 0, D, 5120 + c * 128, 128)
                    wH, rwH = wcols(w_in, 0, D, 6144 + c * 128, 128)
                    wB, rwB = wcols(w_in, 0, D, 4096 + c * 128, 128)
                    mm_group(PS(0), [wC[:, k, lo:lo + 128] for k in range(KC)], [hs[:, k, :] for k in range(KC)], HR + rwC, RPS(0))
                    cp("act", S32(7), PS(0), RPS(0), R(7))
                    mm_group(PS(1), [wH[:, k, lo:lo + 128] for k in range(KC)], [hs[:, k, :] for k in range(KC)], HR + rwH, RPS(1))
                    cp("dve", tbuf[:, 0:2], ccar[:, c, :], ["ccar"], RT)
                    tt(tbuf[:, 2:1026], S32(7), PS(1), ALU.mult, R(7) + RPS(1), RT)
                    cw0 = vecs[:, V_CW + 0 * 8 + c:V_CW + 0 * 8 + c + 1]
                    cw1 = vecs[:, V_CW + 1 * 8 + c:V_CW + 1 * 8 + c + 1]
                    cw2 = vecs[:, V_CW + 2 * 8 + c:V_CW + 2 * 8 + c + 1]
                    ts(S32(6), tbuf[:, 2:1026], cw2, None, ALU.mult, None, RT + ["vecs"], R(6))
                    stt(S32(6), tbuf[:, 1:1025], cw1, S32(6), ALU.mult, ALU.add, RT + R(6) + ["vecs"], R(6))
                    stt(S32(6), tbuf[:, 0:1024], cw0, S32(6), ALU.mult, ALU.add, RT + R(6) + ["vecs"], R(6))
                    cp("dve", ccar[:, c, :], tbuf[:, 1024:1026], RT, ["ccar"])
                    mm_group(PS(2), [wB[:, k, lo:lo + 128] for k in range(KC)], [hs[:, k, :] for k in range(KC)], HR + rwB, RPS(2))
                    tt(S16(c // 2, c % 2), S32(6), PS(2), ALU.mult, R(6) + RPS(2), R(c // 2, c % 2))
            for mb in range(4):
                wv, rw = wcols(w_out, 1024, 1024, mb * 512, 512)
                for ml in range(4):
                    m = mb * 4 + ml
                    pi = next_ps()
                    if m == 0:
                        def fa2(e, pi=pi, wv=wv):
                            last = None
                            for k in range(7):
                                for tb in range(2):
                                    last = e.matmul(PS(pi)[:, tb * 512:(tb + 1) * 512], lhsT=wv[:, k, 0:128],
                                                    rhs=ab[k][:, tb * 512:(tb + 1) * 512], start=(k == 0), stop=False)
                            return last

                        def fb2(e, pi=pi, wv=wv):
                            last = None
                            for tb in range(2):
                                last = e.matmul(PS(pi)[:, tb * 512:(tb + 1) * 512], lhsT=wv[:, 7, 0:128],
                                                rhs=ab[7][:, tb * 512:(tb + 1) * 512], start=False, stop=True)
                            return last
                        P.op("pe", fa2, reads=[r for c in range(7) for r in R(c // 2, c % 2)] + rw, writes=RPS(pi))
                        P.op("pe", fb2, reads=R(3, 1) + rw, writes=RPS(pi))
                    else:
                        mm_group(PS(pi), [wv[:, k, ml * 128:(ml + 1) * 128] for k in range(8)], ab, ABR + rw, RPS(pi))
                    x_add_ps(m, PS(pi), RPS(pi))
                    rms_later(m, 3)
            rms_flush()

        def ffn_block(Wg2, Wu2, Wd2, c0, r0, gate_e=None, tail=False, first=False):
            blk = ffn_block.n
            ffn_block.n += 1
            aslot = 4 + 2 * (blk % 2)
            actv = scr[:, aslot * 1024:(aslot + 2) * 1024].bitcast(BF16).rearrange("p (j t) -> p j t", j=4)
            for half in range(2):
                wgv, rg = wcols(Wg2, 0, D, c0 + half * 256, 256)
                wuv, ru = wcols(Wu2, 0, D, c0 + half * 256, 256)
                for jl in range(2):
                    j = half * 2 + jl
                    lo = jl * 128
                    pa = next_ps()
                    pb = next_ps()
                    if first and j == 0:
                        mm_pair_kpipe(PS(pa), [wgv[:, k, lo:lo + 128] for k in range(KC)], rg,
                                      PS(pb), [wuv[:, k, lo:lo + 128] for k in range(KC)], ru,
                                      [hs[:, k, :] for k in range(KC)], HR, RPS(pa) + RPS(pb))
                    else:
                        mm_group(PS(pa), [wgv[:, k, lo:lo + 128] for k in range(KC)], [hs[:, k, :] for k in range(KC)], HR + rg, RPS(pa))
                        mm_group(PS(pb), [wuv[:, k, lo:lo + 128] for k in range(KC)], [hs[:, k, :] for k in range(KC)], HR + ru, RPS(pb))
                    tslot = 8 + (ffn_block.t % 2)
                    ffn_block.t += 1
                    act(S32(tslot), PS(pa), AF.Silu, RPS(pa), R(tslot))
                    ares = R(aslot + j // 2, j % 2)
                    if gate_e is not None:
                        gbc = S16(gate_e // 2, gate_e % 2)
                        tt(S32(tslot), S32(tslot), gbc, ALU.mult, R(tslot) + R(gate_e // 2, gate_e % 2), R(tslot))
                    tt(actv[:, j, :], S32(tslot), PS(pb), ALU.mult, R(tslot) + RPS(pb), ares)
            wd0, rd0 = wcols(Wd2, r0, 256, 0, D)
            wd1, rd1 = wcols(Wd2, r0 + 256, 256, 0, D)
            ares_all = R(aslot) + R(aslot + 1)
            for m in range(KC):
                pi = next_ps()
                wl = [wd0[:, 0, m * 128:(m + 1) * 128], wd0[:, 1, m * 128:(m + 1) * 128],
                      wd1[:, 0, m * 128:(m + 1) * 128], wd1[:, 1, m * 128:(m + 1) * 128]]
                if m == 0:
                    def fa(e, pi=pi, wl=wl):
                        last = None
                        for j in range(3):
                            for tb in range(2):
                                last = e.matmul(PS(pi)[:, tb * 512:(tb + 1) * 512], lhsT=wl[j], rhs=actv[:, j, tb * 512:(tb + 1) * 512],
                                                start=(j == 0), stop=False)
                        return last

                    def fb(e, pi=pi, wl=wl):
                        last = None
                        for tb in range(2):
                            last = e.matmul(PS(pi)[:, tb * 512:(tb + 1) * 512], lhsT=wl[3], rhs=actv[:, 3, tb * 512:(tb + 1) * 512],
                                            start=False, stop=True)
                        return last
                    P.op("pe", fa, reads=R(aslot) + R(aslot + 1, 0) + rd0 + rd1, writes=RPS(pi))
                    P.op("pe", fb, reads=R(aslot + 1, 1) + rd1, writes=RPS(pi))
                else:
                    mm_group(PS(pi), wl, [actv[:, j, :] for j in range(4)], ares_all + rd0 + rd1, RPS(pi))
                x_add_ps(m, PS(pi), RPS(pi))
                if tail:
                    rms_later(m, 3)
            if tail:
                rms_flush()
        ffn_block.n = 0
        ffn_block.t = 0

        def l0_ffn(hf):
            rms_finish()
            norm_to_h(V_FNE)
            nb = DFF // 512
            for blk in range(nb):
                ffn_block(w_gate, w_up, w_down, blk * 512, blk * 512, tail=(blk == nb - 1), first=(blk == 0))

        def ple(hf, layer):
            rms_finish()
            norm_to_h(V_PLN + layer * 16)
            pT = S16(10).rearrange("p (f t) -> p f t", f=2)
            stg = scr[:, 11 * 1024:13 * 1024].rearrange("p (i f) -> p i f", i=NTILE)
            rs = R(11) + R(12)
            src = p_in[layer, hf * T:(hf + 1) * T, :].rearrange("(i p) f -> p i f", p=128)
            P.dma("sp", "pin", lambda e: [e.dma_start(out=stg, in_=src)], writes=rs)
            for f in range(2):
                pi = next_ps()

                def fn(e, f=f, pi=pi):
                    last = None
                    for ti in range(NTILE):
                        last = e.transpose(PS(pi)[:, ti * 128:(ti + 1) * 128], stg[:, ti, f * 128:(f + 1) * 128], ident)
                    return last
                P.op("pe", fn, reads=rs + ["vecs"], writes=RPS(pi))
                cp("act", pT[:, f, :], PS(pi), RPS(pi), R(10, f))
            wpj = scr[:, 0:2048].bitcast(BF16).rearrange("p (k n) -> p k n", k=2)
            rpjl = R(0) + R(1)
            srcpj = ple_pj[layer].rearrange("(k p) n -> p k n", p=128)
            P.dma("pool", "wpj", lambda e: [e.dma_start(out=wpj, in_=srcpj)], writes=rpjl)
            def ple_evac(m, pa, pb):
                tslot = 8 + (m % 2)
                act(S32(tslot), PS(pa), AF.Sigmoid, RPS(pa), R(tslot))
                tt(S32(tslot), S32(tslot), PS(pb), ALU.mult, R(tslot) + RPS(pb), R(tslot))
                tt(xs[:, m, :], xs[:, m, :], S32(tslot), ALU.add, R(tslot) + [f"x{m}"], [f"x{m}"])
                pend.append(m)

            def ple_pp(m, pb):
                mm_group(PS(pb), [wpj[:, f, m * 128:(m + 1) * 128] for f in range(2)], [pT[:, f, :] for f in range(2)],
                         R(10) + rpjl, RPS(pb))
            for mb in range(8):
                wv, rw = wcols(ple_gw[layer], 0, D, mb * 256, 256)
                if mb == 0:
                    pa0, pa1 = next_ps(), next_ps()
                    mm_pair_kpipe(PS(pa0), [wv[:, k, 0:128] for k in range(KC)], rw,
                                  PS(pa1), [wv[:, k, 128:256] for k in range(KC)], rw,
                                  [hs[:, k, :] for k in range(KC)], HR, RPS(pa0) + RPS(pa1))
                    pb0 = next_ps()
                    ple_pp(0, pb0)
                    ple_evac(0, pa0, pb0)
                    pb1 = next_ps()
                    ple_pp(1, pb1)
                    ple_evac(1, pa1, pb1)
                    continue
                for ml in range(2):
                    m = mb * 2 + ml
                    cur = None
                    if len(pend) >= 2:
                        cur = pend.pop(0)
                        rms_sq(cur)
                    pa = next_ps()
                    pb = next_ps()
                    mm_group(PS(pa), [wv[:, k, ml * 128:(ml + 1) * 128] for k in range(KC)], [hs[:, k, :] for k in range(KC)],
                             HR + rw, RPS(pa))
                    ple_pp(m, pb)
                    ple_evac(m, pa, pb)
                    if cur is not None:
                        rms_mm(cur)
            rms_flush()

        def l1_pool(hf):
            rms_finish()
            def stage_a(c):
                w = 2 << (c // 4)
                i = c % 2
                hnf = S32(4 + i)
                hb = S16(6 + i)[:, 0:1040]
                rf, rb = R(4 + i), R(6 + i)
                cp("act", hb[:, 0:16], pcarb[:, c, :], ["pcarb"], rb)
                stt(hnf, xs[:, c, :], vecs[:, V_MNO + c:V_MNO + c + 1], PS(3), ALU.mult, ALU.mult,
                    [f"x{c}", "vecs"] + RPS(3), rf)
                cp("act", hb[:, 16:1040], hnf, rf, rb)
                cp("act", pcarb[:, c, :], hb[:, 1024:1040], rb, ["pcarb"])
                pi = next_ps()

                def fw(e, pi=pi, w=w, hb=hb):
                    last = None
                    for tb in range(2):
                        for k in range(w):
                            o = 16 + tb * 512 - k
                            last = e.matmul(PS(pi)[:, tb * 512:(tb + 1) * 512], lhsT=identb[:, :], rhs=hb[:, o:o + 512],
                                            start=(k == 0), stop=(k == w - 1))
                    return last
                P.op("pe", fw, reads=rb + ["identb"], writes=RPS(pi))
                return pi

            def stage_b(c, pi):
                g = c // 4
                w = 2 << g
                i = c % 2
                hnf = S32(4 + i)
                rf = R(4 + i)
                stt(hs[:, c, :], PS(pi), 1.0 / w, hnf, ALU.mult, ALU.subtract, RPS(pi) + rf, [f"h{c}"])
                if hf == 0:
                    nfix = w - 1
                    fx = S32(10)[:, 0:16]
                    tt(fx[:, 0:nfix], PS(pi)[:, 0:nfix], vecs[:, V_RF + g * 16:V_RF + g * 16 + nfix], ALU.mult,
                       RPS(pi) + ["vecs"], R(10, 0))
                    tt(hs[:, c, 0:nfix], fx[:, 0:nfix], hnf[:, 0:nfix], ALU.subtract, R(10, 0) + rf, [f"h{c}"])
            pis = {0: stage_a(0)}
            for c in range(KC):
                if c + 1 < KC:
                    pis[c + 1] = stage_a(c + 1)
                stage_b(c, pis[c])
            for g in range(4):
                wv, rw = wcols(pool_w[g], 0, 512, 0, 512)
                for ml in range(4):
                    m = g * 4 + ml
                    pi = next_ps()
                    mm_group(PS(pi), [wv[:, k, ml * 128:(ml + 1) * 128] for k in range(4)], [hs[:, g * 4 + k, :] for k in range(4)],
                             [f"h{g * 4 + k}" for k in range(4)] + rw, RPS(pi))
                    stt(xs[:, m, :], PS(pi), vecs[:, V_PS + m:V_PS + m + 1], xs[:, m, :], ALU.mult, ALU.add,
                        RPS(pi) + [f"x{m}", "vecs"], [f"x{m}"])
                    rms_later(m, 3)
            rms_flush()

        def l1_moe(hf):
            rms_finish(need_sbuf=True)
            norm_to_h(V_FNO)
            XR = [f"x{c}" for c in range(KC)]

            def ftr(e):
                last = None
                for ti in range(NTILE):
                    last = e.transpose(PS(2)[:, ti * 128:(ti + 1) * 128], rstd[:, ti * 128:(ti + 1) * 128], ident)
                return last
            P.op("pe", ftr, reads=["rstd", "vecs"], writes=RPS(2))
            cp("act", rtk[:, :], PS(2).rearrange("p (i c) -> p i c", c=128)[:, :, 0], RPS(2), ["rtk"])

            def fl(e):
                last = None
                for ti in range(NTILE):
                    for c in range(KC):
                        last = e.matmul(psum[:, 2, ti * 8:(ti + 1) * 8], lhsT=xs[:, c, ti * 128:(ti + 1) * 128], rhs=wrg[:, c, :],
                                        start=(c == 0), stop=(c == KC - 1))
                return last
            P.op("pe", fl, reads=XR + ["wrg"], writes=["pb2"])
            tt(lgt[:, :, :], psum[:, 2, 0:64].rearrange("p (i e) -> p i e", e=8),
               rtk[:, :].unsqueeze(2).to_broadcast([128, NTILE, 8]), ALU.mult, ["pb2", "rtk"], ["lgt"])
            L = lgt[:, :, :]
            eq1, l2, eq2, g1, g2, gates = (gsm[:, i, :, :] for i in range(6))
            m1, m2, w1, w2 = (gm[:, i, :] for i in range(4))

            def bc(v):
                return v.unsqueeze(2).to_broadcast([128, NTILE, 8])
            P.op("dve", lambda e: e.tensor_reduce(out=m1, in_=L, axis=AX.X, op=ALU.max), reads=["lgt"], writes=["gm"])
            tt(eq1, L, bc(m1), ALU.is_equal, ["lgt", "gm"], ["gsm"])
            stt(l2, eq1, -1e30, L, ALU.mult, ALU.add, ["gsm", "lgt"], ["gsm"])
            P.op("dve", lambda e: e.tensor_reduce(out=m2, in_=l2, axis=AX.X, op=ALU.max), reads=["gsm"], writes=["gm"])
            tt(eq2, l2, bc(m2), ALU.is_equal, ["gsm", "gm"], ["gsm"])
            tt(w2, m2, m1, ALU.subtract, ["gm"], ["gm"])
            act(w1, w2, AF.Sigmoid, ["gm"], ["gm"], scale=-1.0)
            act(w2, w2, AF.Sigmoid, ["gm"], ["gm"])
            tt(g1, eq1, bc(w1), ALU.mult, ["gsm", "gm"], ["gsm"])
            tt(g2, eq2, bc(w2), ALU.mult, ["gsm", "gm"], ["gsm"])
            tt(gates, g1, g2, ALU.add, ["gsm"], ["gsm"])
            pg = next_ps()

            def fgt(e):
                last = None
                for ti in range(NTILE):
                    last = e.transpose(PS(pg)[0:8, ti * 128:(ti + 1) * 128], gsm[:, 5, ti, :], ident)
                return last
            P.op("pe", fgt, reads=["gsm", "vecs"], writes=RPS(pg))
            gT = S16(10, 0)[0:8, :]
            cp("act", gT, PS(pg)[0:8, :], RPS(pg), R(10, 0))
            for ex in range(NEXP):
                pi = next_ps()

                def fbc(e, pi=pi, ex=ex):
                    e.matmul(PS(pi)[:, 0:512], lhsT=selc[:, ex, :], rhs=gT[:, 0:512], start=True, stop=True)
                    return e.matmul(PS(pi)[:, 512:1024], lhsT=selc[:, ex, :], rhs=gT[:, 512:1024], start=True, stop=True)
                P.op("pe", fbc, reads=R(10, 0) + ["selc"], writes=RPS(pi))
                cp("act", S16(ex // 2, ex % 2), PS(pi), RPS(pi), R(ex // 2, ex % 2))
            for ex in range(NEXP):
                for blk in range(DFE // 512):
                    ffn_block(w_ge[ex], w_ue[ex], w_de[ex], blk * 512, blk * 512, gate_e=ex,
                              tail=(ex == NEXP - 1 and blk == DFE // 512 - 1))

        def store_out(hf, final):
            if final:
                rms_finish()
                for c in range(KC):
                    stt(xs[:, c, :], xs[:, c, :], vecs[:, V_FNG + c:V_FNG + c + 1], PS(3), ALU.mult, ALU.mult,
                        [f"x{c}", "vecs"] + RPS(3), [f"x{c}"])
            for ti in range(NTILE):
                sl = 4 + 2 * (ti % 4)
                stg = scr[:, sl * 1024:(sl + 2) * 1024]
                rs = R(sl) + R(sl + 1)
                for q in range(4):
                    b = next_ps()
                    bank = 2 * b

                    def fn(e, q=q, bank=bank, ti=ti):
                        last = None
                        for j in range(4):
                            c = q * 4 + j
                            last = e.transpose(psum[:, bank, j * 128:(j + 1) * 128], xs[:, c, ti * 128:(ti + 1) * 128], ident)
                        return last
                    P.op("pe", fn, reads=[f"x{c}" for c in range(q * 4, q * 4 + 4)] + ["vecs"], writes=[f"pb{bank}"])
                    cp("act", stg[:, q * 512:(q + 1) * 512], psum[:, bank, :], [f"pb{bank}"], [rs[q]])
                t0 = hf * T + ti * 128
                P.dma("act", f"yout{ti % 4}", lambda e, stg=stg, t0=t0: [e.dma_start(out=y_out[t0:t0 + 128, :], in_=stg)],
                      reads=rs, writes=[f"y{hf}_{ti}"])

        for hf in range(n_halves):
            load_x(hf)
            if stop_after >= 1:
                l0_mixer(hf)
            if stop_after >= 2:
                l0_ffn(hf)
            if stop_after >= 3:
                ple(hf, 0)
            if stop_after >= 4:
                l1_pool(hf)
            if stop_after >= 5:
                l1_moe(hf)
            if stop_after >= 6:
                ple(hf, 1)
            store_out(hf, final=(do_final and stop_after >= 6))
        finals = [P.last_w[f"y{hf}_{ti}"] for hf in range(n_halves) for ti in range(NTILE)]
        fin = {}
        for k, v in finals:
            fin[k] = max(fin.get(k, 0), v)
        P.final_wait("sp", list(fin.items()))

        sems = {}
        for k in P.semkeys:
            sems[k] = es.enter_context(nc.semaphore(k.replace(":", "_")))
        with nc.Block() as block:
            P.emit(block, sems)
    return nc


def pack_vecs(inp):
    v = np.zeros((128, NV), np.float32)

    def pc(a):
        a = np.asarray(a, np.float32).reshape(-1, 128)
        return a.T
    v[:, V_FNG:V_FNG + 16] = pc(inp["final_norm_gain"])
    v[:, V_MNE:V_MNE + 16] = pc(inp["mix_norm_even"][0])
    v[:, V_HG:V_HG + 8] = pc(inp["hgrn_norm_gain"][0])
    for j in range(3):
        v[:, V_CW + j * 8:V_CW + j * 8 + 8] = pc(inp["conv_w"][0, j])
    v[:, V_FNE:V_FNE + 16] = pc(inp["ffn_norm_even"][0])
    v[:, V_MNO:V_MNO + 16] = pc(inp["mix_norm_odd"][0])
    v[:, V_PS:V_PS + 16] = pc(inp["pool_scale"][0])
    v[:, V_FNO:V_FNO + 16] = pc(inp["ffn_norm_odd"][0])
    for l in range(2):
        v[:, V_PLN + l * 16:V_PLN + l * 16 + 16] = pc(inp["ple_norm"][l])
    for r in range(3):
        v[:, V_LB + r * 8:V_LB + r * 8 + 8] = pc(inp["lb_table"][r])
    wr = np.asarray(inp["w_router"][0], np.float32).reshape(16, 128, 8)
    v[:, V_WR:V_WR + 128] = wr.transpose(1, 0, 2).reshape(128, 128)
    v[:, V_ID:V_ID + 128] = np.eye(128, dtype=np.float32)
    s = np.arange(128)[:, None]
    t = np.arange(128)[None, :]
    v[:, V_CM:V_CM + 128] = ((s // 64 == t // 64) & (s <= t)).astype(np.float32)
    for g in range(4):
        w = 2 << g
        v[:, V_RF + g * 16:V_RF + g * 16 + 16] = (1.0 / np.minimum(np.arange(1, 17), w)).astype(np.float32)[None, :]
    return v


_NC_CACHE = {}


def make_in_maps(inp):
    f = lambda a: np.ascontiguousarray(np.asarray(a, dtype=np.float32))
    vecs = pack_vecs(inp)
    shared = dict(
        vecs=vecs,
        w_in=f(inp["w_in_even"][0]), w_out=f(inp["w_out_even"][0]),
        w_gate=f(inp["w_gate_dense"][0]), w_up=f(inp["w_up_dense"][0]), w_down=f(inp["w_down_dense"][0]),
        pool_w=f(inp["pool_w"][0]), w_ge=f(inp["w_gate_exp"][0]), w_ue=f(inp["w_up_exp"][0]), w_de=f(inp["w_down_exp"][0]),
        ple_gw=f(inp["ple_gate_w"]), ple_pj=f(inp["ple_proj"]),
    )
    x = np.asarray(inp["x"], np.float32)
    p = np.asarray(inp["p"], np.float32)
    maps = []
    for b in range(8):
        m = dict(shared)
        m["x_in"] = f(x[b])
        m["p_in"] = f(p[:, b])
        maps.append(m)
    return maps


def kernel(**inputs):
    key = "full"
    if key not in _NC_CACHE:
        _NC_CACHE[key] = build()
    nc = _NC_CACHE[key]
    maps = make_in_maps(inputs)
    res = run_bass_kernel_spmd(nc, maps, core_ids=list(range(8)))
    out = np.stack([np.asarray(res.results[b]["y_out"], dtype=np.float32) for b in range(8)], axis=0)
    return out
```

```python
import numpy as np
from contextlib import ExitStack
import concourse.bass as bass
import concourse.mybir as mybir
from concourse.bass_utils import run_bass_kernel_spmd

F32 = mybir.dt.float32
BF16 = mybir.dt.bfloat16
AF = mybir.ActivationFunctionType
ALU = mybir.AluOpType
AX = mybir.AxisListType

D = 2048
KC = 16
S_FULL = 2048
T = 1024
NTILE = T // 128
EPS = 1e-6
DFF = 5632
NEXP = 8
DFE = 1024

V_FNG, V_MNE, V_HG, V_CW, V_FNE, V_MNO, V_PS, V_FNO, V_PLN, V_LB, V_WR = 0, 16, 32, 40, 64, 80, 96, 112, 128, 160, 184
V_ID, V_CM, V_RF = 312, 440, 568
NV = 632

ENGS = ("pe", "act", "dve", "pool", "sp")


class Prog:
    def __init__(self):
        self.ops = {e: [] for e in ENGS}
        self.count = {}
        self.last_w = {}
        self.readers = {}
        self.waited = {e: {} for e in ENGS}
        self.semkeys = list(ENGS)

    def _deps(self, reads, writes):
        need = {}

        def add(k, v):
            if need.get(k, 0) < v:
                need[k] = v
        for r in reads:
            d = self.last_w.get(r)
            if d is not None:
                add(*d)
        for w in writes:
            d = self.last_w.get(w)
            if d is not None:
                add(*d)
            for k, v in self.readers.get(w, {}).items():
                add(k, v)
        return need

    def _commit(self, reads, writes, me):
        k, v = me
        for r in reads:
            rd = self.readers.setdefault(r, {})
            if rd.get(k, 0) < v:
                rd[k] = v
        for w in writes:
            self.last_w[w] = me
            self.readers[w] = {}

    def _filter(self, eng, need):
        waits = []
        for k, v in need.items():
            if k == "pe" and eng == "pe":
                continue
            if self.waited[eng].get(k, 0) >= v:
                continue
            self.waited[eng][k] = v
            waits.append((k, v))
        return waits

    def op(self, eng, fn, reads=(), writes=()):
        need = self._deps(reads, writes)
        waits = self._filter(eng, need)
        self.count[eng] = self.count.get(eng, 0) + 1
        me = (eng, self.count[eng])
        self._commit(reads, writes, me)
        self.ops[eng].append((waits, fn, (eng, 1)))
        return me

    def dma(self, eng, slot, fn, reads=(), writes=(), n=1):
        key = "dma:" + slot
        if key not in self.semkeys:
            self.semkeys.append(key)
        need = self._deps(reads, writes)
        waits = self._filter(eng, need)
        self.count[key] = self.count.get(key, 0) + 16 * n
        me = (key, self.count[key])
        self._commit(reads, writes, me)
        self.ops[eng].append((waits, fn, (key, 16)))
        return me

    def final_wait(self, eng, deps):
        self.ops[eng].append((list(deps), None, None))

    def emit(self, block, sems):
        def run(engobj, name):
            for waits, fn, inc in self.ops[name]:
                for k, v in waits:
                    engobj.wait_ge(sems[k], v)
                if fn is None:
                    continue
                r = fn(engobj)
                if isinstance(r, (list, tuple)):
                    for ins in r:
                        ins.then_inc(sems[inc[0]], inc[1])
                else:
                    r.then_inc(sems[inc[0]], inc[1])

        @block.tensor
        def _(e):
            run(e, "pe")

        @block.scalar
        def _(e):
            run(e, "act")

        @block.vector
        def _(e):
            run(e, "dve")

        @block.gpsimd
        def _(e):
            run(e, "pool")

        @block.sync
        def _(e):
            run(e, "sp")


def build(stop_after=6, n_halves=2, do_final=True):
    nc = bass.Bass("TRN2", target_bir_lowering=False)

    def din(name, shape):
        return nc.dram_tensor(name, list(shape), F32, kind="ExternalInput").ap()
    x_in = din("x_in", [S_FULL, D])
    p_in = din("p_in", [2, S_FULL, 256])
    vecs_in = din("vecs", [128, NV])
    w_in = din("w_in", [D, 7168])
    w_out = din("w_out", [D, D])
    w_gate = din("w_gate", [D, DFF])
    w_up = din("w_up", [D, DFF])
    w_down = din("w_down", [DFF, D])
    pool_w = din("pool_w", [4, 512, 512])
    w_ge = din("w_ge", [NEXP, D, DFE])
    w_ue = din("w_ue", [NEXP, D, DFE])
    w_de = din("w_de", [NEXP, DFE, D])
    ple_gw = din("ple_gw", [2, D, D])
    ple_pj = din("ple_pj", [2, 256, D])
    y_out = nc.dram_tensor("y_out", [S_FULL, D], F32, kind="ExternalOutput").ap()

    es = ExitStack()
    with es:
        def sb(name, shape, dt):
            return es.enter_context(nc.sbuf_tensor(name, list(shape), dt))
        xs = sb("xs", [128, KC, T], F32)
        hs = sb("hs", [128, KC, T], BF16)
        NWH = 8
        wb = sb("wb", [128, NWH * 2048], BF16)
        NSCR = 14
        scr = sb("scr", [128, NSCR * 1024], F32)
        rstd = sb("rstd", [128, T], F32)
        vecs = sb("vecs_sb", [128, NV], F32)
        Sst = sb("Sst", [128, 8, 128], F32)
        Sb = [sb(f"Sb{i}", [128, 128], BF16) for i in range(4)]
        Stmp = sb("Stmp", [128, 128], F32)
        Pm = [sb(f"Pm{i}", [128, 128], BF16) for i in range(2)]
        identb = sb("identb", [128, 128], BF16)
        onesb = sb("onesb", [128, 128], BF16)
        onesf = sb("onesf", [128, 128], F32)
        rmask = sb("rmask", [128, T], F32)
        lbv = sb("lbv", [128, 3, 8], F32)
        lbd = sb("lbd", [128, 4, 8], F32)
        ccar = sb("ccar", [128, 8, 2], F32)
        pcarb = sb("pcarb", [128, KC, 16], BF16)
        wrg = sb("wrg", [128, KC, 8], F32)
        lgt = sb("lgt", [128, NTILE, 8], F32)
        gsm = sb("gsm", [128, 6, NTILE, 8], F32)
        gm = sb("gm", [128, 4, NTILE], F32)
        rtk = sb("rtk", [128, NTILE], F32)
        selc = sb("selc", [8, NEXP, 128], BF16)
        psum = es.enter_context(nc.psum_tensor("psum", [128, 8, 512], F32))
        psflat = psum[:, :, :].rearrange("p b n -> p (b n)")

        P = Prog()
        ident = vecs[:, V_ID:V_ID + 128]
        cmask = vecs[:, V_CM:V_CM + 128]

        def S32(slot, n=1024, off=0):
            return scr[:, slot * 1024 + off: slot * 1024 + off + n]

        def S16(slot, half=None):
            v = scr[:, slot * 1024:(slot + 1) * 1024].bitcast(BF16)
            if half is None:
                return v
            return v[:, half * 1024:(half + 1) * 1024]

        def R(slot, half=None):
            if half is None:
                return [f"s{slot}a", f"s{slot}b"]
            return [f"s{slot}{'ab'[half]}"]

        def PS(i):
            return psflat[:, i * 1024:(i + 1) * 1024]

        def RPS(i):
            return [f"pb{2 * i}", f"pb{2 * i + 1}"]

        HR = [f"h{c}" for c in range(KC)]
        wstate = {"n": 0}

        def wload(src3, kcn, ncols, after=()):
            size = kcn * ncols
            nhs = (size + 2047) // 2048
            assert nhs in (1, 2)
            if nhs == 2 and wstate["n"] % 2 == 1:
                wstate["n"] += 1
            idx = wstate["n"] % NWH
            wstate["n"] += nhs
            dst = wb[:, idx * 2048: idx * 2048 + size].rearrange("p (k n) -> p k n", k=kcn)
            res = [f"wh{idx + i}" for i in range(nhs)]
            P.dma("pool", f"w{idx}", lambda e: [e.dma_start(out=dst, in_=src3)], reads=list(after), writes=res)
            return dst, res

        def wcols(W2, r0, nrows, c0, ncols, after=()):
            src = W2[r0:r0 + nrows, c0:c0 + ncols].rearrange("(k p) n -> p k n", p=128)
            return wload(src, nrows // 128, ncols, after=after)

        def mm_group(ps, wl, rl, reads, wres):
            n = len(wl)

            def fn(e):
                last = None
                for i in range(n):
                    for tb in range(2):
                        last = e.matmul(ps[:, tb * 512:(tb + 1) * 512], lhsT=wl[i],
                                        rhs=rl[i][:, tb * 512:(tb + 1) * 512],
                                        start=(i == 0), stop=(i == n - 1))
                return last
            P.op("pe", fn, reads=reads, writes=wres)

        def mm_pair_kpipe(psA, wlA, resA, psB, wlB, resB, rl, rnames, wres):
            n = len(rl)
            for k in range(n):
                def fn(e, k=k):
                    last = None
                    for (ps, wl) in ((psA, wlA), (psB, wlB)):
                        for tb in range(2):
                            last = e.matmul(ps[:, tb * 512:(tb + 1) * 512], lhsT=wl[k], rhs=rl[k][:, tb * 512:(tb + 1) * 512],
                                            start=(k == 0), stop=(k == n - 1))
                    return last
                P.op("pe", fn, reads=[rnames[k]] + resA + resB, writes=wres)

        def act(out, in_, func, reads, writes, **kw):
            P.op("act", lambda e: e.activation(out=out, in_=in_, func=func, **kw), reads=reads, writes=writes)

        def tt(out, in0, in1, op, reads, writes, eng="dve"):
            P.op(eng, lambda e: e.tensor_tensor(out=out, in0=in0, in1=in1, op=op), reads=reads, writes=writes)

        def stt(out, in0, scalar, in1, op0, op1, reads, writes):
            P.op("dve", lambda e: e.scalar_tensor_tensor(out=out, in0=in0, scalar=scalar, in1=in1, op0=op0, op1=op1),
                 reads=reads, writes=writes)

        def ts(out, in0, s1, s2, op0, op1, reads, writes, eng="dve"):
            if s2 is None:
                P.op(eng, lambda e: e.tensor_scalar(out=out, in0=in0, scalar1=s1, scalar2=None, op0=op0),
                     reads=reads, writes=writes)
            else:
                P.op(eng, lambda e: e.tensor_scalar(out=out, in0=in0, scalar1=s1, scalar2=s2, op0=op0, op1=op1),
                     reads=reads, writes=writes)

        def cp(eng, out, in_, reads, writes):
            if eng == "act":
                P.op("act", lambda e: e.copy(out=out, in_=in_), reads=reads, writes=writes)
            else:
                P.op(eng, lambda e: e.tensor_copy(out=out, in_=in_), reads=reads, writes=writes)

        def mset(eng, ap, val, writes):
            P.op(eng, lambda e: e.memset(ap, val), writes=writes)

        P.dma("sp", "vecs", lambda e: [e.dma_start(out=vecs[:, :], in_=vecs_in)], writes=["vecs"])
        cp("dve", identb[:, :], ident, ["vecs"], ["identb"])
        mset("dve", onesb[:, :], 1.0, ["onesb"])
        mset("dve", onesf[:, :], 1.0, ["onesf"])
        mset("dve", rmask[:, :], 1.0, ["rmask"])
        mset("dve", rmask[:, :].rearrange("p (c t) -> p c t", t=64)[:, :, 0:1], 0.0, ["rmask"])
        mset("dve", Sst[:, :, :], 0.0, [f"S{h}" for h in range(8)])
        mset("dve", ccar[:, :, :], 0.0, ["ccar"])
        mset("dve", pcarb[:, :, :], 0.0, ["pcarb"])
        lbt = vecs[:, V_LB:V_LB + 24].rearrange("p (r c) -> p r c", r=3)
        act(lbv[:, :, :], lbt, AF.Exp, ["vecs"], ["lbv"])
        tt(lbd[:, 0, :], lbv[:, 0, :], lbv[:, 1, :], ALU.add, ["lbv"], ["lbd"])
        tt(lbd[:, 0, :], lbd[:, 0, :], lbv[:, 2, :], ALU.add, ["lbv", "lbd"], ["lbd"])
        P.op("dve", lambda e: e.reciprocal(out=lbd[:, 1, :], in_=lbd[:, 0, :]), reads=["lbd"], writes=["lbd"])
        tt(lbd[:, 1, :], lbd[:, 1, :], lbv[:, 0, :], ALU.mult, ["lbv", "lbd"], ["lbd"])
        ts(lbd[:, 3, :], lbd[:, 1, :], 1.0, None, ALU.subtract, None, ["lbd"], ["lbd"])
        ts(lbd[:, 2, :], lbd[:, 3, :], -1.0, None, ALU.mult, None, ["lbd"], ["lbd"])
        for ex in range(NEXP):
            cp("dve", selc[:, ex, :], ident[0:8, ex:ex + 1].to_broadcast([8, 128]), ["vecs"], ["selc"])
        wr = vecs[:, V_WR:V_WR + 128].rearrange("p (c e) -> p c e", c=KC)
        for c in range(KC):
            ts(wrg[:, c, :], wr[:, c, :], vecs[:, V_FNO + c:V_FNO + c + 1], None, ALU.mult, None, ["vecs"], ["wrg"])

        def rms_sq(c):
            act(S16(13, c % 2), xs[:, c, :], AF.Square, [f"x{c}"], R(13, c % 2))

        def rms_mm(c):
            ps = PS(3)
            sq = S16(13, c % 2)

            def fn(e):
                last = None
                for tb in range(2):
                    last = e.matmul(ps[:, tb * 512:(tb + 1) * 512], lhsT=onesb[:, :],
                                    rhs=sq[:, tb * 512:(tb + 1) * 512], start=(c == 0), stop=(c == KC - 1))
                return last
            P.op("pe", fn, reads=R(13, c % 2) + ["onesb"], writes=RPS(3))

        def rms_accum(c):
            rms_sq(c)
            rms_mm(c)

        def rms_finish(need_sbuf=False):
            act(rstd[:, :], PS(3), AF.Ln, RPS(3), ["rstd"], scale=1.0 / D, bias=EPS)
            act(PS(3), rstd[:, :], AF.Exp, ["rstd"], RPS(3), scale=-0.5)
            if need_sbuf:
                act(rstd[:, :], rstd[:, :], AF.Exp, ["rstd"], ["rstd"], scale=-0.5)

        pend = []

        def rms_later(m, lag):
            pend.append(m)
            if len(pend) > lag:
                rms_accum(pend.pop(0))

        def rms_flush():
            while pend:
                rms_accum(pend.pop(0))

        def rms_stats(hf):
            for c in range(KC):
                rms_accum(c)
            rms_finish()

        def norm_to_h(gain_off):
            for c in range(KC):
                stt(hs[:, c, :], xs[:, c, :], vecs[:, gain_off + c:gain_off + c + 1], PS(3), ALU.mult, ALU.mult,
                    [f"x{c}", "vecs"] + RPS(3), [f"h{c}"])

        def x_add_ps(m, ps, rps):
            tt(xs[:, m, :], ps, xs[:, m, :], ALU.add, rps + [f"x{m}"], [f"x{m}"])

        pscnt = {"n": 0}

        bkcnt = {"n": 0}

        def next_bank():
            b = bkcnt["n"] % 6
            bkcnt["n"] += 1
            return b

        def next_ps():
            i = pscnt["n"] % 3
            pscnt["n"] += 1
            return i

        def load_x(hf):
            for ti in range(NTILE):
                sl = 4 + 2 * (ti % 4)
                stg = scr[:, sl * 1024:(sl + 2) * 1024]
                rs = R(sl) + R(sl + 1)
                t0 = hf * T + ti * 128
                P.dma("sp", f"xin{ti % 4}", lambda e, stg=stg, t0=t0: [e.dma_start(out=stg, in_=x_in[t0:t0 + 128, :])],
                      writes=rs + (["xgate"] if ti == NTILE - 1 else []))
                for q in range(4):
                    bank = next_bank()

                    def fn(e, q=q, bank=bank, stg=stg):
                        last = None
                        for j in range(4):
                            c = q * 4 + j
                            last = e.transpose(psum[:, bank, j * 128:(j + 1) * 128], stg[:, c * 128:(c + 1) * 128], ident)
                        return last
                    P.op("pe", fn, reads=rs + ["vecs"], writes=[f"pb{bank}"])
                    outv = xs[:, q * 4:(q + 1) * 4, ti * 128:(ti + 1) * 128]
                    inv = psum[:, bank, :].rearrange("p (j t) -> p j t", j=4)
                    cp("act" if q % 2 == 0 else "dve", outv, inv, [f"pb{bank}"], [f"x{c}" for c in range(q * 4, q * 4 + 4)])

        def l0_mixer(hf):
            rms_stats(hf)
            norm_to_h(V_MNE)
            A, B, C, Dd, E = 4, 5, 6, 7, 8
            sring = {"n": 0}
            tail_q = []
            fwd = {}
            for hp in range(4):
                for hl in range(2):
                    hd = hp * 2 + hl
                    lo = 0
                    if hd not in fwd:
                        fwd[hd] = wcols(w_in, 0, D, 1024 + hd * 128, 128, after=["xgate"])
                    wf, rwf = fwd[hd]
                    wq, rwq = wcols(w_in, 0, D, 0 + hd * 128, 128)
                    wi, rwi = wcols(w_in, 0, D, 2048 + hd * 128, 128)
                    wg, rwg = wcols(w_in, 0, D, 3072 + hd * 128, 128)
                    oml = lbd[:, 2, hd:hd + 1]
                    noml = lbd[:, 3, hd:hd + 1]
                    if hd == 0:
                        mm_pair_kpipe(PS(0), [wf[:, k, lo:lo + 128] for k in range(KC)], rwf,
                                      PS(1), [wq[:, k, lo:lo + 128] for k in range(KC)], rwq,
                                      [hs[:, k, :] for k in range(KC)], HR, RPS(0) + RPS(1))
                        act(S32(A), PS(0), AF.Sigmoid, RPS(0), R(A), scale=-1.0)
                    else:
                        act(S32(A), PS(0), AF.Sigmoid, RPS(0), R(A), scale=-1.0)
                        if tail_q:
                            tail_q.pop(0)()
                        mm_group(PS(1), [wq[:, k, lo:lo + 128] for k in range(KC)], [hs[:, k, :] for k in range(KC)],
                                 HR + rwq, RPS(1))
                    mm_group(PS(2), [wi[:, k, lo:lo + 128] for k in range(KC)], [hs[:, k, :] for k in range(KC)],
                             HR + rwi, RPS(2))
                    mm_group(PS(0), [wg[:, k, lo:lo + 128] for k in range(KC)], [hs[:, k, :] for k in range(KC)],
                             HR + rwg, RPS(0))
                    act(S32(B), S32(A), AF.Ln, R(A) + ["lbd"], R(B), scale=noml, bias=1.0)
                    P.op("dve", lambda e: e.tensor_tensor_scan(out=S32(C), data0=rmask[:, :], data1=S32(B), initial=0.0,
                                                               op0=ALU.mult, op1=ALU.add),
                         reads=R(B) + ["rmask"], writes=R(C))
                    c3 = S32(C).rearrange("p (c t) -> p c t", t=64)
                    tt(S32(B).rearrange("p (c t) -> p c t", t=64), c3, c3[:, :, 32:33].to_broadcast([128, 16, 64]),
                       ALU.subtract, R(C), R(B))
                    tt(S32(Dd).rearrange("p (c t) -> p c t", t=64), c3, c3[:, :, 63:64].to_broadcast([128, 16, 64]),
                       ALU.subtract, R(C), R(Dd))
                    act(S32(E), S32(B), AF.Exp, R(B), R(E), scale=-1.0)
                    stt(S16(10, 0), S32(A), oml, S32(E), ALU.mult, ALU.mult, R(A) + R(E) + ["lbd"], R(10, 0))
                    act(S32(E), S32(Dd), AF.Exp, R(Dd), R(E), scale=-1.0)
                    stt(S16(10, 1), S32(A), oml, S32(E), ALU.mult, ALU.mult, R(A) + R(E) + ["lbd"], R(10, 1))
                    act(S32(B), S32(B), AF.Exp, R(B), R(B))
                    act(S32(C), S32(C), AF.Exp, R(C), R(C))
                    cp("act", S16(11, 0), PS(2), RPS(2), R(11, 0))
                    act(S32(A), PS(1), AF.Silu, RPS(1), R(A))
                    tt(S16(9, 0), S32(A), S32(B), ALU.mult, R(A) + R(B), R(9, 0))
                    tt(S16(9, 1), S32(A), S32(C), ALU.mult, R(A) + R(C), R(9, 1))
                    act(S16(hd // 2, hd % 2), PS(0), AF.Silu, RPS(0), R(hd // 2, hd % 2))
                    pv = psum[:, 4, :].bitcast(BF16)
                    pk = psum[:, 5, :].bitcast(BF16)

                    def fnt(e, src, dstp):
                        last = None
                        for ti in range(NTILE):
                            last = e.transpose(dstp[:, ti * 128:(ti + 1) * 128], src[:, ti * 128:(ti + 1) * 128], identb[:, :])
                        return last
                    P.op("pe", lambda e, pv=pv: fnt(e, S16(11, 0), pv), reads=R(11, 0) + ["identb"], writes=["pb4"])
                    P.op("pe", lambda e, pk=pk: fnt(e, S16(10, 1), pk), reads=R(10, 1) + ["identb"], writes=["pb5"])
                    cp("act", S16(11, 1), pv, ["pb4"], R(11, 1))
                    cp("dve", S16(12, 0), pk, ["pb5"], R(12, 0))
                    vtok, kltok = S16(11, 1), S16(12, 0)
                    kdT, qd, qc = S16(10, 0), S16(9, 0), S16(9, 1)
                    eqc = S32(C)
                    r0 = sring["n"] % 4
                    cp("act", Sb[r0][:, :], Sst[:, hd, :], [f"S{hd}"], [f"Sb{r0}"])
                    def core_pe1(ti):
                        tsl = slice(ti * 128, (ti + 1) * 128)
                        psc = psum[:, 4 + ti % 2, 0:128]
                        pkv0 = psum[:, 6, 0:128]
                        pkv1 = psum[:, 7, 0:128]
                        rsc, rkv0, rkv1 = f"pb{4 + ti % 2}", "pb6", "pb7"
                        P.op("pe", lambda e: e.matmul(psc, lhsT=kdT[:, tsl], rhs=qd[:, tsl], start=True, stop=True),
                             reads=R(10, 0) + R(9, 0), writes=[rsc])
                        P.op("pe", lambda e: e.matmul(pkv0, lhsT=kltok[0:64, tsl], rhs=vtok[0:64, tsl], start=True, stop=True),
                             reads=R(12, 0) + R(11, 1), writes=[rkv0])
                        P.op("pe", lambda e: e.matmul(pkv1, lhsT=kltok[64:128, tsl], rhs=vtok[64:128, tsl], start=True, stop=True),
                             reads=R(12, 0) + R(11, 1), writes=[rkv1])

                    def core_dve(ti, r1, r2):
                        psc = psum[:, 4 + ti % 2, 0:128]
                        pkv0 = psum[:, 6, 0:128]
                        pkv1 = psum[:, 7, 0:128]
                        rsc, rkv0, rkv1 = f"pb{4 + ti % 2}", "pb6", "pb7"
                        tt(Pm[ti % 2][:, :], psc, cmask, ALU.mult, [rsc, "vecs"], [f"Pm{ti % 2}"])
                        d0 = eqc[:, ti * 128 + 63:ti * 128 + 64]
                        d1 = eqc[:, ti * 128 + 127:ti * 128 + 128]
                        stt(Stmp[:, :], Sst[:, hd, :], d0, pkv0, ALU.mult, ALU.add, [f"S{hd}", rkv0] + R(C), ["Stmp"])
                        cp("act", Sb[r1][:, :], Stmp[:, :], ["Stmp"], [f"Sb{r1}"])
                        stt(Sst[:, hd, :], Stmp[:, :], d1, pkv1, ALU.mult, ALU.add, ["Stmp", rkv1] + R(C), [f"S{hd}"])
                        cp("act", Sb[r2][:, :], Sst[:, hd, :], [f"S{hd}"], [f"Sb{r2}"])

                    def core_pe2(ti, r0, r1):
                        tsl = slice(ti * 128, (ti + 1) * 128)
                        pso = psum[:, 2 + ti % 2, 0:128]
                        rpo = f"pb{2 + ti % 2}"

                        def fo(e):
                            e.matmul(pso, lhsT=vtok[:, tsl], rhs=Pm[ti % 2][:, :], start=True, stop=False)
                            e.matmul(pso[:, 0:64], lhsT=Sb[r0][:, :], rhs=qc[:, ti * 128:ti * 128 + 64], start=False, stop=False)
                            return e.matmul(pso[:, 64:128], lhsT=Sb[r1][:, :], rhs=qc[:, ti * 128 + 64:ti * 128 + 128],
                                            start=False, stop=True)
                        P.op("pe", fo, reads=R(11, 1) + [f"Pm{ti % 2}", f"Sb{r0}", f"Sb{r1}"] + R(9, 1), writes=[rpo])
                        cp("act", S32(13)[:, tsl], pso, [rpo], R(13))
                    wfn, rwfn = None, None
                    if hd + 1 < 8:
                        fwd[hd + 1] = wcols(w_in, 0, D, 1024 + (hd + 1) * 128, 128)
                        wfn, rwfn = fwd[hd + 1]

                    def fnext(kk, wfn=wfn, rwfn=rwfn):
                        def fn(e):
                            last = None
                            for tb in range(2):
                                last = e.matmul(PS(0)[:, tb * 512:(tb + 1) * 512], lhsT=wfn[:, kk, 0:128],
                                                rhs=hs[:, kk, tb * 512:(tb + 1) * 512], start=(kk == 0), stop=(kk == KC - 1))
                            return last
                        P.op("pe", fn, reads=[f"h{kk}"] + rwfn, writes=RPS(0))
                    core_pe1(0)
                    for ti in range(NTILE):
                        r0 = sring["n"] % 4
                        r1 = (r0 + 1) % 4
                        r2 = (r0 + 2) % 4
                        sring["n"] += 2
                        core_dve(ti, r1, r2)
                        if ti + 1 < NTILE:
                            core_pe1(ti + 1)
                        if hd + 1 < 8:
                            fnext(2 * ti)
                        core_pe2(ti, r0, r1)
                        if hd + 1 < 8:
                            fnext(2 * ti + 1)
                    act(S16(12, 1), S32(13), AF.Square, R(13), R(12, 1))

                    def head_tail(hd=hd):
                        oT = S32(13)

                        def fss(e):
                            e.matmul(PS(3)[:, 0:512], lhsT=onesb[:, :], rhs=S16(12, 1)[:, 0:512], start=True, stop=True)
                            return e.matmul(PS(3)[:, 512:1024], lhsT=onesb[:, :], rhs=S16(12, 1)[:, 512:1024], start=True, stop=True)
                        P.op("pe", fss, reads=R(12, 1) + ["onesb"], writes=RPS(3))
                        act(PS(3), PS(3), AF.Ln, RPS(3), RPS(3), scale=1.0 / 128, bias=EPS)
                        act(PS(3), PS(3), AF.Exp, RPS(3), RPS(3), scale=-0.5)
                        stt(oT, oT, vecs[:, V_HG + hd:V_HG + hd + 1], PS(3), ALU.mult, ALU.mult, R(13) + RPS(3) + ["vecs"], R(13))
                        ao = S16(hd // 2, hd % 2)
                        tt(ao, oT, ao, ALU.mult, R(13) + R(hd // 2, hd % 2), R(hd // 2, hd % 2))
                    tail_q.append(head_tail)
            tail_q.pop(0)()
            ABR = [r for s in range(4) for r in R(s)]
            ab = [S16(c // 2, c % 2) for c in range(8)]
            for mb in range(4):
                wv, rw = wcols(w_out, 0, 1024, mb * 512, 512)
                for ml in range(4):
                    m = mb * 4 + ml
                    pi = next_ps()
                    if m == 0:
                        def fa(e, pi=pi, wv=wv):
                            last = None
                            for k in range(7):
                                for tb in range(2):
                                    last = e.matmul(PS(pi)[:, tb * 512:(tb + 1) * 512], lhsT=wv[:, k, 0:128],
                                                    rhs=ab[k][:, tb * 512:(tb + 1) * 512], start=(k == 0), stop=False)
                            return last

                        def fb(e, pi=pi, wv=wv):
                            last = None
                            for tb in range(2):
                                last = e.matmul(PS(pi)[:, tb * 512:(tb + 1) * 512], lhsT=wv[:, 7, 0:128],
                                                rhs=ab[7][:, tb * 512:(tb + 1) * 512], start=False, stop=True)
                            return last
                        P.op("pe", fa, reads=[r for c in range(7) for r in R(c // 2, c % 2)] + rw, writes=RPS(pi))
                        P.op("pe", fb, reads=R(3, 1) + rw, writes=RPS(pi))
                    else:
                        mm_group(PS(pi), [wv[:, k, ml * 128:(ml + 1) * 128] for k in range(8)], ab, ABR + rw, RPS(pi))
                    x_add_ps(m, PS(pi), RPS(pi))
            tbuf = scr[:, 4 * 1024:4 * 1024 + 1026]
            RT = R(4) + R(5)
            for cpair in range(4):
                for cl in range(2):
                    c = cpair * 2 + cl
                    lo = 0
                    wC, rwC = wcols(w_in, 0, D, 5120 + c * 128, 128)
                    wH, rwH = wcols(w_in, 0, D, 6144 + c * 128, 128)
                    wB, rwB = wcols(w_in, 0, D, 4096 + c * 128, 128)
                    mm_group(PS(0), [wC[:, k, lo:lo + 128] for k in range(KC)], [hs[:, k, :] for k in range(KC)], HR + rwC, RPS(0))
                    cp("act", S32(7), PS(0), RPS(0), R(7))
                    mm_group(PS(1), [wH[:, k, lo:lo + 128] for k in range(KC)], [hs[:, k, :] for k in range(KC)], HR + rwH, RPS(1))
                    cp("dve", tbuf[:, 0:2], ccar[:, c, :], ["ccar"], RT)
                    tt(tbuf[:, 2:1026], S32(7), PS(1), ALU.mult, R(7) + RPS(1), RT)
                    cw0 = vecs[:, V_CW + 0 * 8 + c:V_CW + 0 * 8 + c + 1]
                    cw1 = vecs[:, V_CW + 1 * 8 + c:V_CW + 1 * 8 + c + 1]
                    cw2 = vecs[:, V_CW + 2 * 8 + c:V_CW + 2 * 8 + c + 1]
                    ts(S32(6), tbuf[:, 2:1026], cw2, None, ALU.mult, None, RT + ["vecs"], R(6))
                    stt(S32(6), tbuf[:, 1:1025], cw1, S32(6), ALU.mult, ALU.add, RT + R(6) + ["vecs"], R(6))
                    stt(S32(6), tbuf[:, 0:1024], cw0, S32(6), ALU.mult, ALU.add, RT + R(6) + ["vecs"], R(6))
                    cp("dve", ccar[:, c, :], tbuf[:, 1024:1026], RT, ["ccar"])
                    mm_group(PS(2), [wB[:, k, lo:lo + 128] for k in range(KC)], [hs[:, k, :] for k in range(KC)], HR + rwB, RPS(2))
                    tt(S16(c // 2, c % 2), S32(6), PS(2), ALU.mult, R(6) + RPS(2), R(c // 2, c % 2))
            for mb in range(4):
                wv, rw = wcols(w_out, 1024, 1024, mb * 512, 512)
                for ml in range(4):
                    m = mb * 4 + ml
                    pi = next_ps()
                    if m == 0:
                        def fa2(e, pi=pi, wv=wv):
                            last = None
                            for k in range(7):
                                for tb in range(2):
                                    last = e.matmul(PS(pi)[:, tb * 512:(tb + 1) * 512], lhsT=wv[:, k, 0:128],
                                                    rhs=ab[k][:, tb * 512:(tb + 1) * 512], start=(k == 0), stop=False)
                            return last

                        def fb2(e, pi=pi, wv=wv):
                            last = None
                            for tb in range(2):
                                last = e.matmul(PS(pi)[:, tb * 512:(tb + 1) * 512], lhsT=wv[:, 7, 0:128],
                                                rhs=ab[7][:, tb * 512:(tb + 1) * 512], start=False, stop=True)
                            return last
                        P.op("pe", fa2, reads=[r for c in range(7) for r in R(c // 2, c % 2)] + rw, writes=RPS(pi))
                        P.op("pe", fb2, reads=R(3, 1) + rw, writes=RPS(pi))
                    else:
                        mm_group(PS(pi), [wv[:, k, ml * 128:(ml + 1) * 128] for k in range(8)], ab, ABR + rw, RPS(pi))
                    x_add_ps(m, PS(pi), RPS(pi))
                    rms_later(m, 3)
            rms_flush()

        def ffn_block(Wg2, Wu2, Wd2, c0, r0, gate_e=None, tail=False, first=False):
            blk = ffn_block.n
            ffn_block.n += 1
            aslot = 4 + 2 * (blk % 2)
            actv = scr[:, aslot * 1024:(aslot + 2) * 1024].bitcast(BF16).rearrange("p (j t) -> p j t", j=4)
            for half in range(2):
                wgv, rg = wcols(Wg2, 0, D, c0 + half * 256, 256)
                wuv, ru = wcols(Wu2, 0, D, c0 + half * 256, 256)
                for jl in range(2):
                    j = half * 2 + jl
                    lo = jl * 128
                    pa = next_ps()
                    pb = next_ps()
                    if first and j == 0:
                        mm_pair_kpipe(PS(pa), [wgv[:, k, lo:lo + 128] for k in range(KC)], rg,
                                      PS(pb), [wuv[:, k, lo:lo + 128] for k in range(KC)], ru,
                                      [hs[:, k, :] for k in range(KC)], HR, RPS(pa) + RPS(pb))
                    else:
                        mm_group(PS(pa), [wgv[:, k, lo:lo + 128] for k in range(KC)], [hs[:, k, :] for k in range(KC)], HR + rg, RPS(pa))
                        mm_group(PS(pb), [wuv[:, k, lo:lo + 128] for k in range(KC)], [hs[:, k, :] for k in range(KC)], HR + ru, RPS(pb))
                    tslot = 8 + (ffn_block.t % 2)
                    ffn_block.t += 1
                    act(S32(tslot), PS(pa), AF.Silu, RPS(pa), R(tslot))
                    ares = R(aslot + j // 2, j % 2)
                    if gate_e is not None:
                        gbc = S16(gate_e // 2, gate_e % 2)
                        tt(S32(tslot), S32(tslot), gbc, ALU.mult, R(tslot) + R(gate_e // 2, gate_e % 2), R(tslot))
                    tt(actv[:, j, :], S32(tslot), PS(pb), ALU.mult, R(tslot) + RPS(pb), ares)
            wd0, rd0 = wcols(Wd2, r0, 256, 0, D)
            wd1, rd1 = wcols(Wd2, r0 + 256, 256, 0, D)
            ares_all = R(aslot) + R(aslot + 1)
            for m in range(KC):
                pi = next_ps()
                wl = [wd0[:, 0, m * 128:(m + 1) * 128], wd0[:, 1, m * 128:(m + 1) * 128],
                      wd1[:, 0, m * 128:(m + 1) * 128], wd1[:, 1, m * 128:(m + 1) * 128]]
                if m == 0:
                    def fa(e, pi=pi, wl=wl):
                        last = None
                        for j in range(3):
                            for tb in range(2):
                                last = e.matmul(PS(pi)[:, tb * 512:(tb + 1) * 512], lhsT=wl[j], rhs=actv[:, j, tb * 512:(tb + 1) * 512],
                                                start=(j == 0), stop=False)
                        return last

                    def fb(e, pi=pi, wl=wl):
                        last = None
                        for tb in range(2):
                            last = e.matmul(PS(pi)[:, tb * 512:(tb + 1) * 512], lhsT=wl[3], rhs=actv[:, 3, tb * 512:(tb + 1) * 512],
                                            start=False, stop=True)
                        return last
                    P.op("pe", fa, reads=R(aslot) + R(aslot + 1, 0) + rd0 + rd1, writes=RPS(pi))
                    P.op("pe", fb, reads=R(aslot + 1, 1) + rd1, writes=RPS(pi))
                else:
                    mm_group(PS(pi), wl, [actv[:, j, :] for j in range(4)], ares_all + rd0 + rd1, RPS(pi))
                x_add_ps(m, PS(pi), RPS(pi))
                if tail:
                    rms_later(m, 3)
            if tail:
                rms_flush()
        ffn_block.n = 0
        ffn_block.t = 0

        def l0_ffn(hf):
            rms_finish()
            norm_to_h(V_FNE)
            nb = DFF // 512
            for blk in range(nb):
                ffn_block(w_gate, w_up, w_down, blk * 512, blk * 512, tail=(blk == nb - 1), first=(blk == 0))

        def ple(hf, layer):
            rms_finish()
            norm_to_h(V_PLN + layer * 16)
            pT = S16(10).rearrange("p (f t) -> p f t", f=2)
            stg = scr[:, 11 * 1024:13 * 1024].rearrange("p (i f) -> p i f", i=NTILE)
            rs = R(11) + R(12)
            src = p_in[layer, hf * T:(hf + 1) * T, :].rearrange("(i p) f -> p i f", p=128)
            P.dma("sp", "pin", lambda e: [e.dma_start(out=stg, in_=src)], writes=rs)
            for f in range(2):
                pi = next_ps()

                def fn(e, f=f, pi=pi):
                    last = None
                    for ti in range(NTILE):
                        last = e.transpose(PS(pi)[:, ti * 128:(ti + 1) * 128], stg[:, ti, f * 128:(f + 1) * 128], ident)
                    return last
                P.op("pe", fn, reads=rs + ["vecs"], writes=RPS(pi))
                cp("act", pT[:, f, :], PS(pi), RPS(pi), R(10, f))
            wpj = scr[:, 0:2048].bitcast(BF16).rearrange("p (k n) -> p k n", k=2)
            rpjl = R(0) + R(1)
            srcpj = ple_pj[layer].rearrange("(k p) n -> p k n", p=128)
            P.dma("pool", "wpj", lambda e: [e.dma_start(out=wpj, in_=srcpj)], writes=rpjl)
            def ple_evac(m, pa, pb):
                tslot = 8 + (m % 2)
                act(S32(tslot), PS(pa), AF.Sigmoid, RPS(pa), R(tslot))
                tt(S32(tslot), S32(tslot), PS(pb), ALU.mult, R(tslot) + RPS(pb), R(tslot))
                tt(xs[:, m, :], xs[:, m, :], S32(tslot), ALU.add, R(tslot) + [f"x{m}"], [f"x{m}"])
                pend.append(m)

            def ple_pp(m, pb):
                mm_group(PS(pb), [wpj[:, f, m * 128:(m + 1) * 128] for f in range(2)], [pT[:, f, :] for f in range(2)],
                         R(10) + rpjl, RPS(pb))
            for mb in range(8):
                wv, rw = wcols(ple_gw[layer], 0, D, mb * 256, 256)
                if mb == 0:
                    pa0, pa1 = next_ps(), next_ps()
                    mm_pair_kpipe(PS(pa0), [wv[:, k, 0:128] for k in range(KC)], rw,
                                  PS(pa1), [wv[:, k, 128:256] for k in range(KC)], rw,
                                  [hs[:, k, :] for k in range(KC)], HR, RPS(pa0) + RPS(pa1))
                    pb0 = next_ps()
                    ple_pp(0, pb0)
                    ple_evac(0, pa0, pb0)
                    pb1 = next_ps()
                    ple_pp(1, pb1)
                    ple_evac(1, pa1, pb1)
                    continue
                for ml in range(2):
                    m = mb * 2 + ml
                    cur = None
                    if len(pend) >= 2:
                        cur = pend.pop(0)
                        rms_sq(cur)
                    pa = next_ps()
                    pb = next_ps()
                    mm_group(PS(pa), [wv[:, k, ml * 128:(ml + 1) * 128] for k in range(KC)], [hs[:, k, :] for k in range(KC)],
                             HR + rw, RPS(pa))
                    ple_pp(m, pb)
                    ple_evac(m, pa, pb)
                    if cur is not None:
                        rms_mm(cur)
            rms_flush()

        def l1_pool(hf):
            rms_finish()
            def stage_a(c):
                w = 2 << (c // 4)
                i = c % 2
                hnf = S32(4 + i)
                hb = S16(6 + i)[:, 0:1040]
                rf, rb = R(4 + i), R(6 + i)
                cp("act", hb[:, 0:16], pcarb[:, c, :], ["pcarb"], rb)
                stt(hnf, xs[:, c, :], vecs[:, V_MNO + c:V_MNO + c + 1], PS(3), ALU.mult, ALU.mult,
                    [f"x{c}", "vecs"] + RPS(3), rf)
                cp("act", hb[:, 16:1040], hnf, rf, rb)
                cp("act", pcarb[:, c, :], hb[:, 1024:1040], rb, ["pcarb"])
                pi = next_ps()

                def fw(e, pi=pi, w=w, hb=hb):
                    last = None
                    for tb in range(2):
                        for k in range(w):
                            o = 16 + tb * 512 - k
                            last = e.matmul(PS(pi)[:, tb * 512:(tb + 1) * 512], lhsT=identb[:, :], rhs=hb[:, o:o + 512],
                                            start=(k == 0), stop=(k == w - 1))
                    return last
                P.op("pe", fw, reads=rb + ["identb"], writes=RPS(pi))
                return pi

            def stage_b(c, pi):
                g = c // 4
                w = 2 << g
                i = c % 2
                hnf = S32(4 + i)
                rf = R(4 + i)
                stt(hs[:, c, :], PS(pi), 1.0 / w, hnf, ALU.mult, ALU.subtract, RPS(pi) + rf, [f"h{c}"])
                if hf == 0:
                    nfix = w - 1
                    fx = S32(10)[:, 0:16]
                    tt(fx[:, 0:nfix], PS(pi)[:, 0:nfix], vecs[:, V_RF + g * 16:V_RF + g * 16 + nfix], ALU.mult,
                       RPS(pi) + ["vecs"], R(10, 0))
                    tt(hs[:, c, 0:nfix], fx[:, 0:nfix], hnf[:, 0:nfix], ALU.subtract, R(10, 0) + rf, [f"h{c}"])
            pis = {0: stage_a(0)}
            for c in range(KC):
                if c + 1 < KC:
                    pis[c + 1] = stage_a(c + 1)
                stage_b(c, pis[c])
            for g in range(4):
                wv, rw = wcols(pool_w[g], 0, 512, 0, 512)
                for ml in range(4):
                    m = g * 4 + ml
                    pi = next_ps()
                    mm_group(PS(pi), [wv[:, k, ml * 128:(ml + 1) * 128] for k in range(4)], [hs[:, g * 4 + k, :] for k in range(4)],
                             [f"h{g * 4 + k}" for k in range(4)] + rw, RPS(pi))
                    stt(xs[:, m, :], PS(pi), vecs[:, V_PS + m:V_PS + m + 1], xs[:, m, :], ALU.mult, ALU.add,
                        RPS(pi) + [f"x{m}", "vecs"], [f"x{m}"])
                    rms_later(m, 3)
            rms_flush()

        def l1_moe(hf):
            rms_finish(need_sbuf=True)
            norm_to_h(V_FNO)
            XR = [f"x{c}" for c in range(KC)]

            def ftr(e):
                last = None
                for ti in range(NTILE):
                    last = e.transpose(PS(2)[:, ti * 128:(ti + 1) * 128], rstd[:, ti * 128:(ti + 1) * 128], ident)
                return last
            P.op("pe", ftr, reads=["rstd", "vecs"], writes=RPS(2))
            cp("act", rtk[:, :], PS(2).rearrange("p (i c) -> p i c", c=128)[:, :, 0], RPS(2), ["rtk"])

            def fl(e):
                last = None
                for ti in range(NTILE):
                    for c in range(KC):
                        last = e.matmul(psum[:, 2, ti * 8:(ti + 1) * 8], lhsT=xs[:, c, ti * 128:(ti + 1) * 128], rhs=wrg[:, c, :],
                                        start=(c == 0), stop=(c == KC - 1))
                return last
            P.op("pe", fl, reads=XR + ["wrg"], writes=["pb2"])
            tt(lgt[:, :, :], psum[:, 2, 0:64].rearrange("p (i e) -> p i e", e=8),
               rtk[:, :].unsqueeze(2).to_broadcast([128, NTILE, 8]), ALU.mult, ["pb2", "rtk"], ["lgt"])
            L = lgt[:, :, :]
            eq1, l2, eq2, g1, g2, gates = (gsm[:, i, :, :] for i in range(6))
            m1, m2, w1, w2 = (gm[:, i, :] for i in range(4))

            def bc(v):
                return v.unsqueeze(2).to_broadcast([128, NTILE, 8])
            P.op("dve", lambda e: e.tensor_reduce(out=m1, in_=L, axis=AX.X, op=ALU.max), reads=["lgt"], writes=["gm"])
            tt(eq1, L, bc(m1), ALU.is_equal, ["lgt", "gm"], ["gsm"])
            stt(l2, eq1, -1e30, L, ALU.mult, ALU.add, ["gsm", "lgt"], ["gsm"])
            P.op("dve", lambda e: e.tensor_reduce(out=m2, in_=l2, axis=AX.X, op=ALU.max), reads=["gsm"], writes=["gm"])
            tt(eq2, l2, bc(m2), ALU.is_equal, ["gsm", "gm"], ["gsm"])
            tt(w2, m2, m1, ALU.subtract, ["gm"], ["gm"])
            act(w1, w2, AF.Sigmoid, ["gm"], ["gm"], scale=-1.0)
            act(w2, w2, AF.Sigmoid, ["gm"], ["gm"])
            tt(g1, eq1, bc(w1), ALU.mult, ["gsm", "gm"], ["gsm"])
            tt(g2, eq2, bc(w2), ALU.mult, ["gsm", "gm"], ["gsm"])
            tt(gates, g1, g2, ALU.add, ["gsm"], ["gsm"])
            pg = next_ps()

            def fgt(e):
                last = None
                for ti in range(NTILE):
                    last = e.transpose(PS(pg)[0:8, ti * 128:(ti + 1) * 128], gsm[:, 5, ti, :], ident)
                return last
            P.op("pe", fgt, reads=["gsm", "vecs"], writes=RPS(pg))
            gT = S16(10, 0)[0:8, :]
            cp("act", gT, PS(pg)[0:8, :], RPS(pg), R(10, 0))
            for ex in range(NEXP):
                pi = next_ps()

                def fbc(e, pi=pi, ex=ex):
                    e.matmul(PS(pi)[:, 0:512], lhsT=selc[:, ex, :], rhs=gT[:, 0:512], start=True, stop=True)
                    return e.matmul(PS(pi)[:, 512:1024], lhsT=selc[:, ex, :], rhs=gT[:, 512:1024], start=True, stop=True)
                P.op("pe", fbc, reads=R(10, 0) + ["selc"], writes=RPS(pi))
                cp("act", S16(ex // 2, ex % 2), PS(pi), RPS(pi), R(ex // 2, ex % 2))
            for ex in range(NEXP):
                for blk in range(DFE // 512):
                    ffn_block(w_ge[ex], w_ue[ex], w_de[ex], blk * 512, blk * 512, gate_e=ex,
                              tail=(ex == NEXP - 1 and blk == DFE // 512 - 1))

        def store_out(hf, final):
            if final:
                rms_finish()
                for c in range(KC):
                    stt(xs[:, c, :], xs[:, c, :], vecs[:, V_FNG + c:V_FNG + c + 1], PS(3), ALU.mult, ALU.mult,
                        [f"x{c}", "vecs"] + RPS(3), [f"x{c}"])
            for ti in range(NTILE):
                sl = 4 + 2 * (ti % 4)
                stg = scr[:, sl * 1024:(sl + 2) * 1024]
                rs = R(sl) + R(sl + 1)
                for q in range(4):
                    bank = next_bank()

                    def fn(e, q=q, bank=bank, ti=ti):
                        last = None
                        for j in range(4):
                            c = q * 4 + j
                            last = e.transpose(psum[:, bank, j * 128:(j + 1) * 128], xs[:, c, ti * 128:(ti + 1) * 128], ident)
                        return last
                    P.op("pe", fn, reads=[f"x{c}" for c in range(q * 4, q * 4 + 4)] + ["vecs"], writes=[f"pb{bank}"])
                    cp("act", stg[:, q * 512:(q + 1) * 512], psum[:, bank, :], [f"pb{bank}"], [rs[q]])
                t0 = hf * T + ti * 128
                P.dma("act", f"yout{ti % 4}", lambda e, stg=stg, t0=t0: [e.dma_start(out=y_out[t0:t0 + 128, :], in_=stg)],
                      reads=rs, writes=[f"y{hf}_{ti}"])

        for hf in range(n_halves):
            load_x(hf)
            if stop_after >= 1:
                l0_mixer(hf)
            if stop_after >= 2:
                l0_ffn(hf)
            if stop_after >= 3:
                ple(hf, 0)
            if stop_after >= 4:
                l1_pool(hf)
            if stop_after >= 5:
                l1_moe(hf)
            if stop_after >= 6:
                ple(hf, 1)
            store_out(hf, final=(do_final and stop_after >= 6))
        finals = [P.last_w[f"y{hf}_{ti}"] for hf in range(n_halves) for ti in range(NTILE)]
        fin = {}
        for k, v in finals:
            fin[k] = max(fin.get(k, 0), v)
        P.final_wait("sp", list(fin.items()))

        sems = {}
        for k in P.semkeys:
            sems[k] = es.enter_context(nc.semaphore(k.replace(":", "_")))
        with nc.Block() as block:
            P.emit(block, sems)
    return nc


def pack_vecs(inp):
    v = np.zeros((128, NV), np.float32)

    def pc(a):
        a = np.asarray(a, np.float32).reshape(-1, 128)
        return a.T
    v[:, V_FNG:V_FNG + 16] = pc(inp["final_norm_gain"])
    v[:, V_MNE:V_MNE + 16] = pc(inp["mix_norm_even"][0])
    v[:, V_HG:V_HG + 8] = pc(inp["hgrn_norm_gain"][0])
    for j in range(3):
        v[:, V_CW + j * 8:V_CW + j * 8 + 8] = pc(inp["conv_w"][0, j])
    v[:, V_FNE:V_FNE + 16] = pc(inp["ffn_norm_even"][0])
    v[:, V_MNO:V_MNO + 16] = pc(inp["mix_norm_odd"][0])
    v[:, V_PS:V_PS + 16] = pc(inp["pool_scale"][0])
    v[:, V_FNO:V_FNO + 16] = pc(inp["ffn_norm_odd"][0])
    for l in range(2):
        v[:, V_PLN + l * 16:V_PLN + l * 16 + 16] = pc(inp["ple_norm"][l])
    for r in range(3):
        v[:, V_LB + r * 8:V_LB + r * 8 + 8] = pc(inp["lb_table"][r])
    wr = np.asarray(inp["w_router"][0], np.float32).reshape(16, 128, 8)
    v[:, V_WR:V_WR + 128] = wr.transpose(1, 0, 2).reshape(128, 128)
    v[:, V_ID:V_ID + 128] = np.eye(128, dtype=np.float32)
    s = np.arange(128)[:, None]
    t = np.arange(128)[None, :]
    v[:, V_CM:V_CM + 128] = ((s // 64 == t // 64) & (s <= t)).astype(np.float32)
    for g in range(4):
        w = 2 << g
        v[:, V_RF + g * 16:V_RF + g * 16 + 16] = (1.0 / np.minimum(np.arange(1, 17), w)).astype(np.float32)[None, :]
    return v


_NC_CACHE = {}


def make_in_maps(inp):
    f = lambda a: np.ascontiguousarray(np.asarray(a, dtype=np.float32))
    vecs = pack_vecs(inp)
    shared = dict(
        vecs=vecs,
        w_in=f(inp["w_in_even"][0]), w_out=f(inp["w_out_even"][0]),
        w_gate=f(inp["w_gate_dense"][0]), w_up=f(inp["w_up_dense"][0]), w_down=f(inp["w_down_dense"][0]),
        pool_w=f(inp["pool_w"][0]), w_ge=f(inp["w_gate_exp"][0]), w_ue=f(inp["w_up_exp"][0]), w_de=f(inp["w_down_exp"][0]),
        ple_gw=f(inp["ple_gate_w"]), ple_pj=f(inp["ple_proj"]),
    )
    x = np.asarray(inp["x"], np.float32)
    p = np.asarray(inp["p"], np.float32)
    maps = []
    for b in range(8):
        m = dict(shared)
        m["x_in"] = f(x[b])
        m["p_in"] = f(p[:, b])
        maps.append(m)
    return maps


def kernel(**inputs):
    key = "full"
    if key not in _NC_CACHE:
        _NC_CACHE[key] = build()
    nc = _NC_CACHE[key]
    maps = make_in_maps(inputs)
    res = run_bass_kernel_spmd(nc, maps, core_ids=list(range(8)))
    out = np.stack([np.asarray(res.results[b]["y_out"], dtype=np.float32) for b in range(8)], axis=0)
    return out
```
